# Optimizing a Trainium2 kernel written in Bass

```python
import math
import numpy as np
import jax
import jax.numpy as jnp
from jax import lax

D_MODEL = 1024
BATCH = 16
SEQ = 2048
DEPTH = 4

GROUP_W = D_MODEL // 4

GLA_H = 4
GLA_DV = GROUP_W // GLA_H
GLA_DK = GLA_DV // 2
GLA_RANK = 16
GLA_TAU = 16.0
GLA_CHUNK = 16

SGU_G = 4
SGU_CG = GROUP_W // SGU_G
SGU_CHUNK = 128

SSM_H = 4
SSM_P = GROUP_W // SSM_H
SSM_G = 2
SSM_N = 64
SSM_CONV = 4
SSM_CHUNK = 128
SSM_CONV_CH = GROUP_W + 2 * SSM_G * SSM_N

DIL_H = 4
DIL_DH = GROUP_W // DIL_H
ROT_DIM = DIL_DH // 4
ROPE_THETA = 500000.0
DIL_BRANCHES = ((128, 1), (512, 4), (2048, 16))

N_EXPERTS = 128
TOP_K = 8
N_EXPERT_GROUPS = 8
TOPK_GROUPS = 4
D_EXPERT = 256
ROUTED_SCALE = 1.0
MOE_BLOCK = 256

ALPHA = (2 * DEPTH) ** 0.25
BETA = (8 * DEPTH) ** -0.25
LN_EPS = 1e-5
RMS_EPS = 1e-6

IN_SPLITS = (GLA_H * GLA_DK, GLA_H * GLA_DK, GROUP_W, GROUP_W, GLA_RANK,
             GROUP_W, GROUP_W,
             GROUP_W, SSM_CONV_CH, SSM_H,
             GROUP_W, GROUP_W, GROUP_W)
N_IN = sum(IN_SPLITS)
IN_OFFSETS = [int(o) for o in np.cumsum(IN_SPLITS)[:-1]]

kernel_name = 'hybrid_parallel_heads_moe_deepnorm'


def _layernorm(x, g, b):
    xf = x.astype(jnp.float32)
    mu = jnp.mean(xf, axis=-1, keepdims=True)
    var = jnp.mean(jnp.square(xf - mu), axis=-1, keepdims=True)
    return ((xf - mu) * lax.rsqrt(var + LN_EPS) * g + b).astype(x.dtype)


def _rmsnorm(x, w):
    xf = x.astype(jnp.float32)
    return (xf * lax.rsqrt(jnp.mean(xf * xf, axis=-1, keepdims=True) + RMS_EPS) * w).astype(x.dtype)


def _gla_mixer(q, k, v, r, lr, w_gate, b_gate, norm_w):
    dt_in = q.dtype
    Bn, S, _ = q.shape
    f32 = jnp.float32
    C = GLA_CHUNK
    nc = S // C
    gk = jax.nn.log_sigmoid((lr @ w_gate + b_gate).astype(f32)) / GLA_TAU

    def chunk(t, d):
        return t.reshape(Bn, nc, C, GLA_H, d).transpose(0, 3, 1, 2, 4).astype(f32)

    qc = chunk(q, GLA_DK) * GLA_DK ** -0.5
    kc = chunk(k, GLA_DK)
    vc = chunk(v, GLA_DV)
    bc = jnp.cumsum(chunk(gk, GLA_DK), axis=3)
    causal = jnp.tril(jnp.ones((C, C), dtype=bool))
    diff = bc[..., :, None, :] - bc[..., None, :, :]
    decay = jnp.exp(jnp.where(causal[:, :, None], diff, -jnp.inf))
    attn = jnp.einsum('bhnid,bhnjd,bhnijd->bhnij', qc, kc, decay)
    o_intra = jnp.einsum('bhnij,bhnjv->bhniv', attn, vc)

    b_last = bc[..., -1:, :]
    q_in = qc * jnp.exp(bc)
    k_in = kc * jnp.exp(b_last - bc)
    d_state = jnp.einsum('bhnjd,bhnjv->nbhdv', k_in, vc)
    chunk_decay = jnp.exp(b_last[..., 0, :]).transpose(2, 0, 1, 3)

    def step(state, inp):
        dec, ds = inp
        return state * dec[..., None] + ds, state

    _, prev = lax.scan(step, jnp.zeros((Bn, GLA_H, GLA_DK, GLA_DV), f32), (chunk_decay, d_state))
    o_inter = jnp.einsum('bhnid,nbhdv->bhniv', q_in, prev)
    o = (o_intra + o_inter).transpose(0, 2, 3, 1, 4).reshape(Bn, S, GLA_H, GLA_DV)
    o = _rmsnorm(o, norm_w).reshape(Bn, S, GROUP_W) * jax.nn.silu(r.astype(f32))
    return o.astype(dt_in)


def _sgu_mixer(u, v, ln_g, ln_b, w_s, b_s):
    Bn, S, _ = u.shape
    nc = S // SGU_CHUNK
    u = jax.nn.gelu(u, approximate=False)
    v = _layernorm(jax.nn.gelu(v, approximate=False), ln_g, ln_b)
    vc = v.reshape(Bn, nc, SGU_CHUNK, SGU_G, SGU_CG)
    w = w_s * jnp.tril(jnp.ones((SGU_CHUNK, SGU_CHUNK), w_s.dtype))
    s = jnp.einsum('gij,bnjgc->bnigc', w, vc) + b_s.T[:, :, None]
    return u * s.reshape(Bn, S, GROUP_W)


def _ssd_chunked(X, dA, Bh, Ch):
    Bn, S, H, P = X.shape
    L = SSM_CHUNK
    nc = S // L
    X = X.reshape(Bn, nc, L, H, P)
    Bh = Bh.reshape(Bn, nc, L, H, SSM_N)
    Ch = Ch.reshape(Bn, nc, L, H, SSM_N)
    A = dA.reshape(Bn, nc, L, H).transpose(0, 3, 1, 2)
    Acs = jnp.cumsum(A, axis=-1)
    tril = jnp.tril(jnp.ones((L, L), dtype=bool))
    Lm = jnp.exp(jnp.where(tril, Acs[..., :, None] - Acs[..., None, :], -jnp.inf))
    y_diag = jnp.einsum('bclhn,bcshn,bhcls,bcshp->bclhp', Ch, Bh, Lm, X)
    decay_states = jnp.exp(Acs[..., -1:] - Acs)
    states = jnp.einsum('bclhn,bhcl,bclhp->bchpn', Bh, decay_states, X)
    cs = jnp.cumsum(jnp.pad(Acs[..., -1], ((0, 0), (0, 0), (1, 0))), axis=-1)
    trc = jnp.tril(jnp.ones((nc + 1, nc + 1), dtype=bool))
    decay_chunk = jnp.exp(jnp.where(trc, cs[..., :, None] - cs[..., None, :], -jnp.inf))
    states = jnp.concatenate([jnp.zeros_like(states[:, :1]), states], axis=1)
    new_states = jnp.einsum('bhzc,bchpn->bzhpn', decay_chunk, states)
    y_off = jnp.einsum('bclhn,bchpn,bhcl->bclhp', Ch, new_states[:, :-1], jnp.exp(Acs))
    return (y_diag + y_off).reshape(Bn, S, H, P)


def _ssd_mixer(z, xbc, dt, conv_w, conv_b, dt_bias, a_log, d_skip, norm_w):
    dt_in = z.dtype
    f32 = jnp.float32
    Bn, S, _ = z.shape
    xbc = lax.conv_general_dilated(xbc, conv_w[:, None, :], window_strides=(1,),
                                   padding=((SSM_CONV - 1, 0),),
                                   dimension_numbers=('NWC', 'WIO', 'NWC'),
                                   feature_group_count=SSM_CONV_CH)
    xbc = jax.nn.silu(xbc + conv_b)
    xs, Bm, Cm = jnp.split(xbc, [GROUP_W, GROUP_W + SSM_G * SSM_N], axis=-1)
    rep = SSM_H // SSM_G
    xs = xs.reshape(Bn, S, SSM_H, SSM_P).astype(f32)
    Bh = jnp.repeat(Bm.reshape(Bn, S, SSM_G, SSM_N), rep, axis=2).astype(f32)
    Ch = jnp.repeat(Cm.reshape(Bn, S, SSM_G, SSM_N), rep, axis=2).astype(f32)
    dt = jax.nn.softplus((dt + dt_bias).astype(f32))
    A = -jnp.exp(a_log.astype(f32))
    y = _ssd_chunked(xs * dt[..., None], dt * A, Bh, Ch)
    y = y + d_skip.astype(f32)[:, None] * xs
    y = y.reshape(Bn, S, GROUP_W) * jax.nn.silu(z.astype(f32))
    y = _rmsnorm(y.reshape(Bn, S, SSM_G, GROUP_W // SSM_G), norm_w.reshape(SSM_G, -1))
    return y.reshape(Bn, S, GROUP_W).astype(dt_in)


def _rope_partial(t, cos, sin):
    half = ROT_DIM // 2
    x1 = t[..., :half]
    x2 = t[..., half:ROT_DIM]
    c = cos[:, :, None, :]
    s = sin[:, :, None, :]
    return jnp.concatenate([x1 * c - x2 * s, x2 * c + x1 * s, t[..., ROT_DIM:]], axis=-1)


def _banded_attention(q, k, v, span):
    N, L, H, dh = q.shape
    nb = L // span
    qb = q.reshape(N, nb, span, H, dh)
    kb = k.reshape(N, nb, span, H, dh)
    vb = v.reshape(N, nb, span, H, dh)
    prev = lambda t: jnp.pad(t, ((0, 0), (1, 0), (0, 0), (0, 0), (0, 0)))[:, :-1]
    kk = jnp.concatenate([prev(kb), kb], axis=2)
    vv = jnp.concatenate([prev(vb), vb], axis=2)
    s = jnp.einsum('nbqhd,nbkhd->nbhqk', qb, kk).astype(jnp.float32) * dh ** -0.5
    i = jnp.arange(span)[:, None]
    j = jnp.arange(2 * span)[None, :]
    band = (j >= i) & (j <= i + span)
    has_prev = (jnp.arange(nb) > 0)[:, None, None] | (j >= span)[None]
    mask = band[None] & has_prev
    s = jnp.where(mask[None, :, None], s, -jnp.inf)
    m = jnp.max(s, axis=-1, keepdims=True)
    p = jnp.exp(s - m)
    den = jnp.sum(p, axis=-1, keepdims=True)
    o = jnp.einsum('nbhqk,nbkhd->nbqhd', p / den, vv.astype(jnp.float32))
    lse = (m + jnp.log(den))[..., 0]
    return o.reshape(N, L, H, dh), lse.transpose(0, 1, 3, 2).reshape(N, L, H)


def _dilated_branch(q, k, v, dil, span):
    Bn, S, H, dh = q.shape
    L = S // dil
    Lp = -(-L // span) * span

    def sub(t):
        t = t.reshape(Bn, L, dil, H, dh).transpose(0, 2, 1, 3, 4).reshape(Bn * dil, L, H, dh)
        return jnp.pad(t, ((0, 0), (0, Lp - L), (0, 0), (0, 0)))

    o, lse = _banded_attention(sub(q), sub(k), sub(v), span)
    o = o[:, :L].reshape(Bn, dil, L, H, dh).transpose(0, 2, 1, 3, 4).reshape(Bn, S, H, dh)
    lse = lse[:, :L].reshape(Bn, dil, L, H).transpose(0, 2, 1, 3).reshape(Bn, S, H)
    return o, lse


def _dilated_mixer(q, k, v, cos, sin):
    dt_in = q.dtype
    Bn, S, _ = q.shape
    q = _rope_partial(q.reshape(Bn, S, DIL_H, DIL_DH), cos, sin)
    k = _rope_partial(k.reshape(Bn, S, DIL_H, DIL_DH), cos, sin)
    v = v.reshape(Bn, S, DIL_H, DIL_DH)
    outs, lses = [], []
    for window, dil in DIL_BRANCHES:
        o, lse = _dilated_branch(q, k, v, dil, window // dil)
        outs.append(o)
        lses.append(lse)
    w = jax.nn.softmax(jnp.stack(lses, axis=0), axis=0)
    o = jnp.einsum('rbsh,rbshd->bshd', w, jnp.stack(outs, axis=0))
    return o.reshape(Bn, S, GROUP_W).astype(dt_in)


def _token_mixer(x, w_in, gla_w_gate, gla_b_gate, gla_norm_w, sgu_ln_g, sgu_ln_b, sgu_w, sgu_b,
                 ssm_conv_w, ssm_conv_b, ssm_dt_bias, ssm_a_log, ssm_d, ssm_norm_w, w_out, cos, sin):
    proj = x @ w_in
    (a_q, a_k, a_v, a_r, a_lr, b_u, b_v, c_z, c_xbc, c_dt, d_q, d_k, d_v) = jnp.split(proj, IN_OFFSETS, axis=-1)
    ya = _gla_mixer(a_q, a_k, a_v, a_r, a_lr, gla_w_gate, gla_b_gate, gla_norm_w)
    yb = _sgu_mixer(b_u, b_v, sgu_ln_g, sgu_ln_b, sgu_w, sgu_b).astype(x.dtype)
    yc = _ssd_mixer(c_z, c_xbc, c_dt, ssm_conv_w, ssm_conv_b, ssm_dt_bias, ssm_a_log, ssm_d, ssm_norm_w)
    yd = _dilated_mixer(d_q, d_k, d_v, cos, sin)
    y = jnp.concatenate([ya, yb, yc, yd], axis=-1).astype(x.dtype)
    return y @ w_out


def _swiglu(h, wg, wu, wd):
    return (jax.nn.silu(h @ wg) * (h @ wu)) @ wd


def _moe(h, router_w, router_bias, w_gate, w_up, w_down, sh_gate, sh_up, sh_down):
    Bn, S, Dm = h.shape
    T = Bn * S
    f32 = jnp.float32
    hf = h.reshape(T, Dm)
    scores = jax.nn.sigmoid((hf @ router_w).astype(f32))
    choice = scores + router_bias.astype(f32)
    per_group = N_EXPERTS // N_EXPERT_GROUPS
    grp_score = jnp.sum(lax.top_k(choice.reshape(T, N_EXPERT_GROUPS, per_group), 2)[0], axis=-1)
    _, gidx = lax.top_k(grp_score, TOPK_GROUPS)
    gmask = jnp.sum(jax.nn.one_hot(gidx, N_EXPERT_GROUPS, dtype=f32), axis=1) > 0
    choice = jnp.where(jnp.repeat(gmask, per_group, axis=1), choice, -jnp.inf)
    _, idx = lax.top_k(choice, TOP_K)
    gate = jnp.take_along_axis(scores, idx, axis=1)
    gate = gate / jnp.sum(gate, axis=-1, keepdims=True) * ROUTED_SCALE

    n_assign = T * TOP_K
    flat_e = idx.reshape(-1)
    order = jnp.argsort(flat_e)
    e_sorted = flat_e[order]
    tok_sorted = (order // TOP_K).astype(jnp.int32)
    g_sorted = gate.reshape(-1)[order]
    counts = jnp.bincount(flat_e, length=N_EXPERTS)
    padded = (counts + MOE_BLOCK - 1) // MOE_BLOCK * MOE_BLOCK
    pad_end = jnp.cumsum(padded)
    pad_start = pad_end - padded
    start = jnp.cumsum(counts) - counts
    dest = pad_start[e_sorted] + jnp.arange(n_assign) - start[e_sorted]
    n_blocks = -(-n_assign // MOE_BLOCK) + N_EXPERTS
    slot_tok = jnp.zeros((n_blocks * MOE_BLOCK,), jnp.int32).at[dest].set(tok_sorted)
    slot_gate = jnp.zeros((n_blocks * MOE_BLOCK,), f32).at[dest].set(g_sorted)
    blk_expert = jnp.minimum(jnp.searchsorted(pad_end, jnp.arange(n_blocks) * MOE_BLOCK, side='right'),
                             N_EXPERTS - 1)

    def body(acc, inp):
        tok, g, e = inp
        y = _swiglu(hf[tok], w_gate[e], w_up[e], w_down[e])
        return acc.at[tok].add((y * g[:, None]).astype(acc.dtype)), None

    routed, _ = lax.scan(body, jnp.zeros_like(hf),
                         (slot_tok.reshape(n_blocks, MOE_BLOCK), slot_gate.reshape(n_blocks, MOE_BLOCK), blk_expert))
    shared = _swiglu(hf, sh_gate, sh_up, sh_down)
    return (routed + shared).reshape(Bn, S, Dm)


def setup_inputs(seed: int = 0) -> dict:
    key = jax.random.key(seed)
    k = jax.random.split(key, 32)
    f32 = jnp.float32
    L = DEPTH

    def nrm(kk, shape, scale):
        return jax.random.normal(kk, shape, f32) * scale

    dt0 = jnp.exp(jax.random.uniform(k[10], (L, SSM_H), f32, math.log(1e-3), math.log(1e-1)))
    return {
        'x': nrm(k[0], (BATCH, SEQ, D_MODEL), 1.0),
        'positions': jnp.arange(SEQ, dtype=jnp.int32)[None, :]
                     + jax.random.randint(k[1], (BATCH, 1), 0, 4096, dtype=jnp.int32),
        'w_in': nrm(k[2], (L, D_MODEL, N_IN), D_MODEL ** -0.5),
        'gla_w_gate': nrm(k[3], (L, GLA_RANK, GLA_H * GLA_DK), GLA_RANK ** -0.5),
        'gla_b_gate': nrm(k[4], (L, GLA_H * GLA_DK), 0.1),
        'gla_norm_w': 1.0 + nrm(k[5], (L, GLA_DV), 0.02),
        'sgu_ln_g': 1.0 + nrm(k[6], (L, GROUP_W), 0.02),
        'sgu_ln_b': nrm(k[7], (L, GROUP_W), 0.02),
        'sgu_w': nrm(k[8], (L, SGU_G, SGU_CHUNK, SGU_CHUNK), SGU_CHUNK ** -0.5),
        'sgu_b': 1.0 + nrm(k[9], (L, SGU_G, SGU_CHUNK), 0.02),
        'ssm_conv_w': nrm(k[11], (L, SSM_CONV, SSM_CONV_CH), SSM_CONV ** -0.5),
        'ssm_conv_b': nrm(k[12], (L, SSM_CONV_CH), 0.02),
        'ssm_dt_bias': dt0 + jnp.log(-jnp.expm1(-dt0)),
        'ssm_a_log': jnp.log(jax.random.uniform(k[13], (L, SSM_H), f32, 1.0, 16.0)),
        'ssm_d': 1.0 + nrm(k[14], (L, SSM_H), 0.02),
        'ssm_norm_w': 1.0 + nrm(k[15], (L, GROUP_W), 0.02),
        'w_out': nrm(k[16], (L, D_MODEL, D_MODEL), D_MODEL ** -0.5 * BETA),
        'ln1_g': 1.0 + nrm(k[17], (L, D_MODEL), 0.02),
        'ln1_b': nrm(k[18], (L, D_MODEL), 0.02),
        'router_w': nrm(k[19], (L, D_MODEL, N_EXPERTS), D_MODEL ** -0.5),
        'router_bias': nrm(k[20], (L, N_EXPERTS), 0.01),
        'exp_w_gate': nrm(k[21], (L, N_EXPERTS, D_MODEL, D_EXPERT), D_MODEL ** -0.5),
        'exp_w_up': nrm(k[22], (L, N_EXPERTS, D_MODEL, D_EXPERT), D_MODEL ** -0.5),
        'exp_w_down': nrm(k[23], (L, N_EXPERTS, D_EXPERT, D_MODEL), D_EXPERT ** -0.5 * BETA),
        'sh_w_gate': nrm(k[24], (L, D_MODEL, D_EXPERT), D_MODEL ** -0.5),
        'sh_w_up': nrm(k[25], (L, D_MODEL, D_EXPERT), D_MODEL ** -0.5),
        'sh_w_down': nrm(k[26], (L, D_EXPERT, D_MODEL), D_EXPERT ** -0.5 * BETA),
        'ln2_g': 1.0 + nrm(k[27], (L, D_MODEL), 0.02),
        'ln2_b': nrm(k[28], (L, D_MODEL), 0.02),
    }


def reference(x, positions, w_in, gla_w_gate, gla_b_gate, gla_norm_w, sgu_ln_g, sgu_ln_b, sgu_w, sgu_b,
              ssm_conv_w, ssm_conv_b, ssm_dt_bias, ssm_a_log, ssm_d, ssm_norm_w, w_out, ln1_g, ln1_b,
              router_w, router_bias, exp_w_gate, exp_w_up, exp_w_down, sh_w_gate, sh_w_up, sh_w_down,
              ln2_g, ln2_b):
    inv_freq = ROPE_THETA ** (-jnp.arange(0, ROT_DIM, 2, dtype=jnp.float32) / ROT_DIM)
    ang = positions.astype(jnp.float32)[..., None] * inv_freq
    cos, sin = jnp.cos(ang), jnp.sin(ang)
    for l in range(DEPTH):
        y = _token_mixer(x, w_in[l], gla_w_gate[l], gla_b_gate[l], gla_norm_w[l], sgu_ln_g[l], sgu_ln_b[l],
                         sgu_w[l], sgu_b[l], ssm_conv_w[l], ssm_conv_b[l], ssm_dt_bias[l], ssm_a_log[l],
                         ssm_d[l], ssm_norm_w[l], w_out[l], cos, sin)
        x = _layernorm(ALPHA * x + y, ln1_g[l], ln1_b[l])
        y = _moe(x, router_w[l], router_bias[l], exp_w_gate[l], exp_w_up[l], exp_w_down[l],
                 sh_w_gate[l], sh_w_up[l], sh_w_down[l])
        x = _layernorm(ALPHA * x + y, ln2_g[l], ln2_b[l])
    return x
```

```python
import math
import numpy as np
from contextlib import ExitStack
import concourse.bass as bass
import concourse.mybir as mybir

F32 = mybir.dt.float32
BF16 = mybir.dt.bfloat16
I32 = mybir.dt.int32
AF = mybir.ActivationFunctionType
ALU = mybir.AluOpType
AX = mybir.AxisListType

D = 1024
KD = 8
NIN = 2836
NE = 128
DEPTH = 4
ALPHA = (2 * DEPTH) ** 0.25
LN_EPS = 1e-5
RMS_EPS = 1e-6
O_AQ, O_AK, O_AV, O_AR, O_ALR = 0, 128, 256, 512, 768
O_BU, O_BV = 784, 1040
O_CZ, O_CXBC, O_CDT = 1296, 1552, 2064
O_DQ, O_DK, O_DV = 2068, 2324, 2580

PV = {}
_o = 0
for _n, _w in [("ln1_g", 1024), ("ln1_b", 1024), ("ln2_g", 1024), ("ln2_b", 1024),
               ("sgu_g", 256), ("sgu_b", 256), ("gla_nw", 256), ("ssm_nw", 256), ("ssm_d", 256),
               ("dt_bias", 4), ("a_log", 4), ("r_bias", 128), ("sgu_bs", 256)]:
    PV[_n] = (_o, _w)
    _o += _w
NPV = _o
PSM0 = PV["sgu_g"][0]
NPSM = NPV - PSM0
NPC = 1 + 16 + 4
CO = {}
_o = 0
for _n, _w in [("ident", 128), ("triu", 128), ("negmask", 128), ("dilmask", 256), ("invf", 8), ("selden", 64)]:
    CO[_n] = (_o, _w)
    _o += _w
NCONST = _o


def make_consts():
    c = np.zeros((128, NCONST), np.float32)
    p = np.arange(128)[:, None]
    f = np.arange(128)[None, :]
    c[:, CO["ident"][0]:CO["ident"][0] + 128] = (p == f)
    c[:, CO["triu"][0]:CO["triu"][0] + 128] = (p <= f)
    c[:, CO["negmask"][0]:CO["negmask"][0] + 128] = np.where(p <= f, 0.0, -30000.0)
    o = CO["dilmask"][0]
    c[:, o:o + 128] = np.where(p <= f, 0.0, -30000.0)
    c[:, o + 128:o + 256] = np.where(p >= f, 0.0, -30000.0)
    inv_freq = (500000.0 ** (-np.arange(0, 16, 2, dtype=np.float32) / np.float32(16))).astype(np.float32)
    c[:, CO["invf"][0]:CO["invf"][0] + 8] = inv_freq[None, :]
    return c


class Buf:
    __slots__ = ("name", "h", "last_w", "readers", "excl")

    def __init__(self, name, h, excl=False):
        self.name = name
        self.h = h
        self.excl = excl
        self.last_w = None
        self.readers = {}

    def __getitem__(self, idx):
        return self.h[idx]


class Sched:
    def __init__(self, nc, stack):
        self.nc = nc
        self.stack = stack
        self.eng = {"pe": nc.tensor, "dve": nc.vector, "act": nc.scalar, "pool": nc.gpsimd, "sp": nc.sync}
        self.sem = {}
        self.cnt = {}
        self.seen = {}
        for e in self.eng:
            self.sem[e] = stack.enter_context(nc.semaphore("s_" + e))
            self.cnt[e] = 0
            self.seen[e] = {}
        self.nwaits = 0
        self.dma_i = {}
        self.ninstr = 0

    def sbuf(self, name, shape, dtype):
        h = self.stack.enter_context(self.nc.sbuf_tensor(name, list(shape), dtype))
        return Buf(name, h)

    def psum(self, name, shape, dtype):
        h = self.stack.enter_context(self.nc.psum_tensor(name, list(shape), dtype))
        return Buf(name, h, excl=True)

    def dram(self, name, shape, dtype, kind="Internal"):
        h = self.nc.dram_tensor(name, list(shape), dtype, kind=kind)
        return Buf(name, h.ap())

    def dma_stream(self, name):
        return "dma_" + name

    def _wait(self, eng, deps):
        e = self.eng[eng]
        seen = self.seen[eng]
        for s, v in deps.items():
            if v <= 0:
                continue
            if eng == "pe" and s == "pe":
                continue
            if s.startswith("dma_"):
                v = self.cnt[s]
            if seen.get(s, 0) >= v:
                continue
            e.wait_ge(self.sem[s], v)
            seen[s] = v
            self.nwaits += 1

    def _deps(self, reads, writes):
        deps = {}

        def add(sv):
            if sv is None:
                return
            s, v = sv
            if deps.get(s, 0) < v:
                deps[s] = v
        for b in reads:
            add(b.last_w)
            if b.excl:
                for s, v in b.readers.items():
                    add((s, v))
        for b in writes:
            add(b.last_w)
            for s, v in b.readers.items():
                add((s, v))
        return deps

    def op(self, eng, fn, reads=(), writes=()):
        deps = self._deps(reads, writes)
        self._wait(eng, deps)
        ins = fn()
        self.cnt[eng] += 1
        ins.then_inc(self.sem[eng], 1)
        c = self.cnt[eng]
        for b in reads:
            b.readers[eng] = c
        for b in writes:
            b.last_w = (eng, c)
            b.readers = {}
        self.ninstr += 1
        return ins

    def dma(self, q, stream, out_ap, in_ap, reads=(), writes=(), rot=4, chain=True, **kw):
        n = self.dma_i.get(stream, 0)
        self.dma_i[stream] = n + 1
        sub = f"{stream}#{n % rot}"
        if sub not in self.sem:
            self.sem[sub] = self.stack.enter_context(self.nc.semaphore("s_" + sub.replace("#", "_")))
            self.cnt[sub] = 0
        deps = self._deps(reads, writes)
        if chain and self.cnt[sub] > 0:
            deps[sub] = self.cnt[sub]
        self._wait(q, deps)
        ins = self.eng[q].dma_start(out=out_ap, in_=in_ap, **kw)
        self.cnt[sub] += 16
        ins.then_inc(self.sem[sub], 16)
        c = self.cnt[sub]
        for b in reads:
            b.readers[sub] = c
        for b in writes:
            b.last_w = (sub, c)
            b.readers = {}
        self.ninstr += 1
        return ins

    def finish(self, out_bufs, eng="sp"):
        deps = {s: v for s, v in self.cnt.items() if v > 0}
        self._wait(eng, deps)


class Rot:
    def __init__(self, bufs):
        self.bufs = bufs
        self.i = 0

    def next(self):
        b = self.bufs[self.i % len(self.bufs)]
        self.i += 1
        return b


class Cfg:
    def __init__(self, T=2048, NSEQ=2, NL=4, n_exp=129, mixers=("gla", "sgu", "ssd", "dil"), dbg=False):
        self.T = T
        self.NT = T // 128
        self.NG = T // 512
        self.NSEQ = NSEQ
        self.NL = NL
        self.n_exp = n_exp
        self.mixers = mixers
        self.dbg = dbg


class MK:
    def __init__(self, cfg):
        self.cfg = cfg

    def bank(self):
        b = self.PS[self.ps_i % 8]
        self.ps_i += 1
        return b

    def tmp(self, name, shape, dtype, n=2, stack=None):
        st = stack if stack is not None else self.st
        bufs = []
        for i in range(n):
            nm = self.uniq(f"{name}{i}")
            h = st.enter_context(self.nc.sbuf_tensor(nm, list(shape), dtype))
            bufs.append(Buf(nm, h))
        return Rot(bufs)

    def uniq(self, name):
        self._uid = getattr(self, "_uid", 0) + 1
        return f"{name}_u{self._uid}"

    def alloc(self, name, shape, dtype, stack=None):
        st = stack if stack is not None else self.st
        name = self.uniq(name)
        h = st.enter_context(self.nc.sbuf_tensor(name, list(shape), dtype))
        return Buf(name, h)

    def barrier(self):
        S = self.S
        deps = {s: v for s, v in S.cnt.items() if v > 0}
        for e in ("pe", "dve", "act", "pool", "sp"):
            S._wait(e, dict(deps))

    def mm(self, out, lhsT, rhs, start, stop, reads, writes):
        nc = self.nc
        return self.S.op("pe", lambda: nc.tensor.matmul(out, lhsT=lhsT, rhs=rhs, start=start, stop=stop),
                         reads=reads, writes=writes)

    def tr(self, out, in_, reads, writes):
        nc = self.nc
        idn = self.ident
        n = in_.shape[0]
        return self.S.op("pe", lambda: nc.tensor.transpose(out=out, in_=in_, identity=idn[0:n, 0:n]),
                         reads=list(reads) + [self.CONST], writes=writes)

    def act(self, out, in_, func, reads, writes, **kw):
        nc = self.nc
        return self.S.op("act", lambda: nc.scalar.activation(out=out, in_=in_, func=func, **kw),
                         reads=reads, writes=writes)

    def tt(self, out, in0, in1, op, reads, writes, eng="dve"):
        e = self.S.eng[eng]
        return self.S.op(eng, lambda: e.tensor_tensor(out=out, in0=in0, in1=in1, op=op), reads=reads, writes=writes)

    def ts(self, out, in0, s1, op0, reads, writes, s2=None, op1=None, eng="dve", **kw):
        e = self.S.eng[eng]
        if op1 is None:
            return self.S.op(eng, lambda: e.tensor_scalar(out=out, in0=in0, scalar1=s1, scalar2=None, op0=op0, **kw),
                             reads=reads, writes=writes)
        return self.S.op(eng, lambda: e.tensor_scalar(out=out, in0=in0, scalar1=s1, scalar2=s2, op0=op0, op1=op1, **kw),
                         reads=reads, writes=writes)

    def stt(self, out, in0, scalar, in1, op0, op1, reads, writes, eng="dve"):
        e = self.S.eng[eng]
        return self.S.op(eng, lambda: e.scalar_tensor_tensor(out=out, in0=in0, scalar=scalar, in1=in1, op0=op0, op1=op1),
                         reads=reads, writes=writes)

    def cp(self, out, in_, reads, writes, eng="dve"):
        if eng == "act":
            return self.act(out, in_, AF.Copy, reads, writes)
        e = self.S.eng[eng]
        return self.S.op(eng, lambda: e.tensor_copy(out=out, in_=in_), reads=reads, writes=writes)

    def memset(self, ap, val, writes, eng="dve"):
        e = self.S.eng[eng]
        return self.S.op(eng, lambda: e.memset(ap, val), writes=writes)

    def load(self, out_ap, in_ap, writes, stream="ld", q="sp", reads=()):
        return self.S.dma(q, self.S.dma_stream(stream), out_ap, in_ap, reads=reads, writes=writes)

    def loadw(self, out_ap, in_ap, writes, stream, rot=2, chain=True):
        return self.S.dma("pool", self.S.dma_stream(stream), out_ap, in_ap, writes=writes, rot=rot, chain=chain)

    def build(self):
        cfg = self.cfg
        nc = bass.Bass("TRN2", target_bir_lowering=False)
        self.nc = nc
        T, NT, NSEQ, NL = cfg.T, cfg.NT, cfg.NSEQ, cfg.NL

        def din(name, shape, dt=F32):
            return nc.dram_tensor(name, list(shape), dt, kind="ExternalInput").ap()
        self.d_x = din("x", [NSEQ * T, D])
        self.d_pos = din("pos", [NSEQ * 128, NT], I32)
        self.d_win = din("w_in", [NL * D, NIN])
        self.d_wout = din("w_out", [NL * D, D])
        self.d_rw = din("router_w", [NL * D, NE])
        import os
        tiny = os.environ.get("STOP", "") in ("mix", "load")
        self.d_wg = din("exp_w_gate", [8 if tiny else NL * NE * D, 256])
        self.d_wu = din("exp_w_up", [8 if tiny else NL * NE * D, 256])
        self.d_wd = din("exp_w_down", [8 if tiny else NL * NE * 256, D])
        self.d_swg = din("sh_w_gate", [NL * D, 256])
        self.d_swu = din("sh_w_up", [NL * D, 256])
        self.d_swd = din("sh_w_down", [NL * 256, D])
        self.d_wgate = din("gla_w_gate", [NL * 16, 128])
        self.d_sguwT = din("sgu_wT", [NL * 4 * 128, 128])
        self.d_pvec = din("pvec", [NL * 128, NPV])
        self.d_pcol = din("pcol", [NL * 128, NPC])
        self.d_const = din("consts", [128, NCONST])
        self.d_out = nc.dram_tensor("out", [NSEQ * T, D], F32, kind="ExternalOutput").ap()
        self.dbg = {}
        if cfg.dbg:
            for nm, shp in [("dbg_yT", [D, T]), ("dbg_h", [T, D]), ("dbg_G", [T, NE])]:
                self.dbg[nm] = nc.dram_tensor(nm, shp, F32, kind="ExternalOutput").ap()

        with ExitStack() as st:
            self.st = st
            S = Sched(nc, st)
            self.S = S
            self.PS = [S.psum(f"ps{i}", [128, 512], F32) for i in range(8)]
            self.ps_i = 0
            self.Xh = st.enter_context(nc.sbuf_tensor("X", [128, NT, D], F32))
            self.X = [Buf(f"X{t}", self.Xh[:, t, :]) for t in range(NT)]
            self.XTh = st.enter_context(nc.sbuf_tensor("XT", [128, KD, T], BF16))
            self.XT = [Buf(f"XT{g}", self.XTh[:, :, g * 512:(g + 1) * 512]) for g in range(cfg.NG)]
            self.CONST = self.alloc("CONST", [128, NCONST], F32)
            self.PSM = self.alloc("PSM", [128, NPSM], F32)
            self.PCOL = self.alloc("PCOL", [128, NPC], F32)
            self.Gh = st.enter_context(nc.sbuf_tensor("G", [128, NT, NE + 1], F32))
            self.G = Buf("G", self.Gh)
            self.memset(self.Gh[:, :, :], 1.0, writes=[self.G])
            self.setup_consts()
            import os
            self.stop = os.environ.get("STOP", "")
            for s in range(NSEQ):
                self.load_seq(s)
                if self.stop == "load":
                    break
                for l in range(NL):
                    self.layer(s, l)
            S.finish([])
            print("instrs", S.ninstr, "waits", S.nwaits, {k: v for k, v in S.cnt.items()})
        return nc

    def cst(self, name):
        o, w = CO[name]
        return self.CONST[:, o:o + w]

    def psm(self, name):
        o, w = PV[name]
        return self.PSM[:, o - PSM0:o - PSM0 + w]

    def setup_consts(self):
        self.load(self.CONST[:], self.d_const[:, :], writes=[self.CONST])
        self.ident = self.cst("ident")
        self.IDB = self.alloc("IDB", [128, 128], BF16)
        self.cp(self.IDB[:], self.ident, reads=[self.CONST], writes=[self.IDB])
        self.DMASK = self.alloc("DMASK", [128, 256], BF16)
        self.cp(self.DMASK[:], self.cst("dilmask"), reads=[self.CONST], writes=[self.DMASK])

    def load_seq(self, s):
        cfg = self.cfg
        for t in range(cfg.NT):
            r0 = s * cfg.T + t * 128
            self.load(self.X[t][:], self.d_x[r0:r0 + 128, :], writes=[self.X[t]], stream="xin")
            self.transpose_tile(t)
        if "dil" in cfg.mixers:
            self.rope_tables(s)

    def transpose_tile(self, t, htf=None):
        g = t // 4
        c0 = (t % 4) * 128
        for half in range(2):
            ps = self.bank()
            for j in range(4):
                k = half * 4 + j
                self.tr(ps[:, j * 128:(j + 1) * 128], self.X[t][:, k * 128:(k + 1) * 128], reads=[self.X[t]], writes=[ps])
            src = ps[:, :].rearrange("p (a b) -> p a b", a=4)
            dst = self.XT[g][:, half * 4:half * 4 + 4, c0:c0 + 128]
            if half == 0:
                self.cp(dst, src, reads=[ps], writes=[self.XT[g]], eng="act")
            else:
                self.cp(dst, src, reads=[ps], writes=[self.XT[g]], eng="dve")
            if htf is not None:
                self.cp(htf[:, half * 4:half * 4 + 4, :], src, reads=[ps], writes=[htf], eng=("dve" if half == 0 else "act"))

    def proj_tok(self, t, W, c0, n, ps_ap, ps):
        g = t // 4
        tc0 = (t % 4) * 128
        for k in range(KD):
            self.mm(ps_ap, self.XT[g][:, k, tc0:tc0 + 128], W[:, k, c0:c0 + n], k == 0, k == KD - 1,
                    reads=[self.XT[g], W], writes=[ps])

    def proj_feat(self, g, W, c0, m, ps_ap, ps):
        for k in range(KD):
            self.mm(ps_ap, W[:, k, c0:c0 + m], self.XT[g][:, k, :], k == 0, k == KD - 1,
                    reads=[self.XT[g], W], writes=[ps])

    def win_ap(self, l, c0, c1):
        return self.d_win[l * D:(l + 1) * D, :].rearrange("(k p) n -> p k n", p=128)[:, :, c0:c1]

    def layer(self, s, l):
        cfg = self.cfg
        nc = self.nc
        self.load(self.PSM[:], self.d_pvec[l * 128:(l + 1) * 128, PSM0:NPV], writes=[self.PSM])
        self.load(self.PCOL[:], self.d_pcol[l * 128:(l + 1) * 128, :], writes=[self.PCOL])
        with ExitStack() as mst:
            YTh = mst.enter_context(nc.sbuf_tensor(self.uniq("YT"), [128, KD, cfg.T], BF16))
            self.YTh = YTh
            self.YT = [Buf(f"YT{m}", YTh[:, 2 * m:2 * m + 2, :]) for m in range(4)]
            names = ["gla", "sgu", "ssd", "dil"]
            for m, nm in enumerate(names):
                if nm not in cfg.mixers:
                    self.memset(self.YT[m][:], 0.0, writes=[self.YT[m]], eng="pool")
                    continue
                with ExitStack() as sst:
                    getattr(self, "mix_" + nm)(s, l, sst, self.YT[m])
                    self.barrier()
            if cfg.dbg and s == 0 and l == 0:
                self.dump_yT()
            if self.stop == "mix":
                return
            with ExitStack() as sst:
                self.outproj_ln_router(s, l, sst)
                self.barrier()
        if self.stop == "outproj":
            return
        with ExitStack() as sst:
            self.moe(s, l, sst)
            self.barrier()

    def dump_yT(self):
        cfg = self.cfg
        with ExitStack() as sst:
            tmp = self.alloc("dbgy", [128, cfg.T], F32, stack=sst)
            for k in range(KD):
                self.cp(tmp[:], self.YTh[:, k, :], reads=[self.YT[k // 2]], writes=[tmp])
                self.load(self.dbg["dbg_yT"][k * 128:(k + 1) * 128, :], tmp[:], writes=[], reads=[tmp], stream="dbg")
            self.barrier()


    def layernorm(self, src, dst, g_ap, b_ap, n, eps, reads, preads, writes, st, tmpn):
        nc = self.nc
        self.S.op("dve", lambda: nc.vector.reduce_sum(out=st[:, 0:1], in_=src, axis=AX.X), reads=reads, writes=[st])
        self.ts(st[:, 1:2], st[:, 0:1], -1.0 / n, ALU.mult, reads=[st], writes=[st])
        self.act(tmpn[:, 0:n], src, AF.Square, reads=list(reads) + [st], writes=[tmpn, st], bias=st[:, 1:2], scale=1.0,
                 accum_out=st[:, 2:3])
        self.act(st[:, 3:4], st[:, 2:3], AF.Sqrt, reads=[st], writes=[st], scale=1.0 / n, bias=float(eps))
        self.S.op("dve", lambda: nc.vector.reciprocal(out=st[:, 4:5], in_=st[:, 3:4]), reads=[st], writes=[st])
        self.ts(tmpn[:, 0:n], src, st[:, 1:2], ALU.add, reads=list(reads) + [st], writes=[tmpn], s2=st[:, 4:5], op1=ALU.mult)
        self.tt(tmpn[:, 0:n], tmpn[:, 0:n], g_ap, ALU.mult, reads=[tmpn] + list(preads), writes=[tmpn])
        self.tt(dst, tmpn[:, 0:n], b_ap, ALU.add, reads=[tmpn] + list(preads), writes=writes)

    def mix_gla(self, s, l, st, YTm):
        cfg = self.cfg
        nc = self.nc
        W = self.alloc("Wgla", [128, KD, 784], BF16, stack=st)
        self.loadw(W[:], self.win_ap(l, 0, 784), writes=[W], stream="win")
        WGf = self.alloc("WGf", [16, 128], F32, stack=st)
        self.load(WGf[:], self.d_wgate[l * 16:(l + 1) * 16, :], writes=[WGf])
        NBG = self.alloc("NBG", [128, 1], F32, stack=st)
        self.ts(NBG[:], self.PCOL[:, 0:1], -1.0, ALU.mult, reads=[self.PCOL], writes=[NBG])
        Qbd = self.alloc("Qbd", [128, 4, 512], BF16, stack=st)
        self.memset(Qbd[:], 0.0, writes=[Qbd], eng="pool")
        QT = self.alloc("glQT", [128, 512], F32, stack=st)
        KTf = self.alloc("glKTf", [128, 512], F32, stack=st)
        LR = self.alloc("glLR", [16, 512], F32, stack=st)
        SP = self.alloc("glSP", [128, 512], F32, stack=st)
        CS = self.alloc("glCS", [128, 512], F32, stack=st)
        EQ = self.alloc("glEQ", [128, 512], F32, stack=st)
        EK = self.alloc("glEK", [128, 512], F32, stack=st)
        KT = self.alloc("glKT", [128, 512], BF16, stack=st)
        NCL = self.alloc("glNCL", [128, 4], F32, stack=st)
        EKI = self.tmp("glEKI", [128, 128], F32, stack=st)
        KINr = self.tmp("glKIN", [128, 128], BF16, stack=st)
        S32 = self.alloc("glS32", [128, 64], F32, stack=st)
        Sb = self.alloc("glSb", [128, 64], BF16, stack=st)
        self.memset(S32[:], 0.0, writes=[S32])
        self.memset(Sb[:], 0.0, writes=[Sb])
        Vr = self.tmp("glV", [128, 256], BF16, stack=st)
        SRr = self.tmp("glSR", [128, 256], F32, stack=st)
        ATr = self.tmp("glAT", [128, 512], BF16, stack=st)
        OSr = self.tmp("glOS", [128, 256], F32, stack=st)
        SQr = self.tmp("glSQ", [128, 256], F32, stack=st)
        STr = self.tmp("glST", [128, 8], F32, stack=st)
        Yr = self.tmp("glY", [128, 256], F32, stack=st)
        triu4 = self.cst("triu").unsqueeze(1).to_broadcast([128, 4, 128])
        for g in range(cfg.NG):
            psq, psk, psl = self.bank(), self.bank(), self.bank()
            self.proj_feat(g, W, O_AQ, 128, psq[:, :], psq)
            self.proj_feat(g, W, O_AK, 128, psk[:, :], psk)
            self.proj_feat(g, W, O_ALR, 16, psl[0:16, :], psl)
            self.cp(QT[:], psq[:, :], reads=[psq], writes=[QT], eng="act")
            self.cp(KTf[:], psk[:, :], reads=[psk], writes=[KTf], eng="dve")
            self.cp(LR[:], psl[0:16, :], reads=[psl], writes=[LR], eng="act")
            psz = self.bank()
            self.mm(psz[:, :], WGf[:], LR[:], True, True, reads=[WGf, LR], writes=[psz])
            self.act(SP[:], psz[:, :], AF.Exp, reads=[psz, NBG], writes=[SP], scale=-1.0, bias=NBG[:, 0:1])
            self.act(SP[:], SP[:], AF.Ln, reads=[SP], writes=[SP], bias=1.0)
            for c in range(4):
                sl = slice(c * 128, (c + 1) * 128)
                self.S.op("dve", lambda: nc.vector.tensor_tensor_scan(out=CS[:, sl], data0=SP[:, sl], data1=SP[:, sl], initial=0.0,
                                                                     op0=ALU.add, op1=ALU.bypass), reads=[SP], writes=[CS])
            self.act(EQ[:], CS[:], AF.Exp, reads=[CS], writes=[EQ], scale=-1.0 / 16)
            self.act(EK[:], CS[:], AF.Exp, reads=[CS], writes=[EK], scale=1.0 / 16)
            for h in range(4):
                ps_ = slice(32 * h, 32 * h + 32)
                self.stt(Qbd[ps_, h, :], QT[ps_, :], 32.0 ** -0.5, EQ[ps_, :], ALU.mult, ALU.mult, reads=[QT, EQ], writes=[Qbd])
            self.tt(KT[:], KTf[:], EK[:], ALU.mult, reads=[KTf, EK], writes=[KT])
            self.ts(NCL[:].unsqueeze(2), CS[:, :].rearrange("p (c i) -> p c i", c=4)[:, :, 127:128], -1.0 / 16, ALU.mult,
                    reads=[CS], writes=[NCL])
            for c in range(4):
                t = 4 * g + c
                sl = slice(c * 128, (c + 1) * 128)
                psvr = self.bank()
                self.proj_tok(t, W, O_AV, 512, psvr[:, :], psvr)
                V, SR = Vr.next(), SRr.next()
                self.cp(V[:], psvr[:, 0:256], reads=[psvr], writes=[V], eng="dve")
                self.act(SR[:], psvr[:, 256:512], AF.Silu, reads=[psvr], writes=[SR])
                eki, KIN = EKI.next(), KINr.next()
                self.act(eki[:], CS[:, sl], AF.Exp, reads=[CS, NCL], writes=[eki], scale=1.0 / 16, bias=NCL[:, c:c + 1])
                self.tt(eki[:], eki[:], KTf[:, sl], ALU.mult, reads=[eki, KTf], writes=[eki])
                pst = self.bank()
                self.tr(pst[:, 0:128], eki[:], reads=[eki], writes=[pst])
                self.cp(KIN[:], pst[:, 0:128], reads=[pst], writes=[KIN], eng="act")
                psa = self.bank()
                self.mm(psa[:, :].rearrange("p (h i) -> p h i", h=4), KT[:, sl], Qbd[:, :, sl], True, True, reads=[KT, Qbd], writes=[psa])
                AT = ATr.next()
                self.tt(AT[:, :].rearrange("p (h i) -> p h i", h=4), psa[:, :].rearrange("p (h i) -> p h i", h=4), triu4, ALU.mult,
                        reads=[psa, self.CONST], writes=[AT])
                pso = self.bank()
                for h in range(4):
                    self.mm(pso[:, h * 64:(h + 1) * 64], AT[:, h * 128:(h + 1) * 128], V[:, h * 64:(h + 1) * 64], True, False,
                            reads=[AT, V], writes=[pso])
                    self.mm(pso[:, h * 64:(h + 1) * 64], Qbd[:, h, sl], Sb[:], False, True, reads=[Qbd, Sb], writes=[pso])
                pss = self.bank()
                self.mm(pss[:, 0:256], KIN[:], V[:], True, True, reads=[KIN, V], writes=[pss])
                for h in range(4):
                    ps_ = slice(32 * h, 32 * h + 32)
                    self.stt(S32[ps_, :], S32[ps_, :], EQ[ps_, c * 128 + 127:c * 128 + 128], pss[ps_, h * 64:(h + 1) * 64],
                             ALU.mult, ALU.add, reads=[S32, EQ, pss], writes=[S32])
                self.cp(Sb[:], S32[:], reads=[S32], writes=[Sb], eng="pool")
                OS, SQ, ST, Y = OSr.next(), SQr.next(), STr.next(), Yr.next()
                self.cp(OS[:], pso[:, 0:256], reads=[pso], writes=[OS], eng="act")
                self.tt(SQ[:], OS[:], OS[:], ALU.mult, reads=[OS], writes=[SQ], eng="pool")
                self.S.op("dve", lambda: nc.vector.tensor_reduce(out=ST[:, 0:4], in_=SQ[:, :].rearrange("p (h v) -> p h v", h=4),
                                                                 axis=AX.X, op=ALU.add), reads=[SQ], writes=[ST])
                self.act(ST[:, 0:4], ST[:, 0:4], AF.Sqrt, reads=[ST], writes=[ST], scale=1.0 / 64, bias=float(RMS_EPS))
                self.S.op("dve", lambda: nc.vector.reciprocal(out=ST[:, 4:8], in_=ST[:, 0:4]), reads=[ST], writes=[ST])
                self.tt(Y[:, :].rearrange("p (h v) -> p h v", h=4), OS[:, :].rearrange("p (h v) -> p h v", h=4),
                        ST[:, 4:8].unsqueeze(2).to_broadcast([128, 4, 64]), ALU.mult, reads=[OS, ST], writes=[Y])
                self.tt(Y[:], Y[:], self.psm("gla_nw"), ALU.mult, reads=[Y, self.PSM], writes=[Y])
                self.tt(Y[:], Y[:], SR[:], ALU.mult, reads=[Y, SR], writes=[Y])
                self.emit_yT(Y, YTm, t)

    def mix_sgu(self, s, l, st, YTm):
        cfg = self.cfg
        nc = self.nc
        W = self.alloc("Wsgu", [128, KD, 512], BF16, stack=st)
        self.loadw(W[:], self.win_ap(l, O_BU, O_BU + 512), writes=[W], stream="win")
        WSf = self.alloc("WSf", [128, 4, 128], F32, stack=st)
        WS = self.alloc("WS", [128, 4, 128], BF16, stack=st)
        self.load(WSf[:], self.d_sguwT[l * 512:(l + 1) * 512, :].rearrange("(g j) i -> j g i", j=128), writes=[WSf])
        self.tt(WS[:], WSf[:], self.cst("triu").unsqueeze(1).to_broadcast([128, 4, 128]), ALU.mult,
                reads=[WSf, self.CONST], writes=[WS])
        Ub = self.tmp("sgU", [128, 256], F32, stack=st)
        Vg = self.tmp("sgV", [128, 256], F32, stack=st)
        Vt = self.tmp("sgT", [128, 256], F32, stack=st)
        VNb = self.tmp("sgN", [128, 256], BF16, stack=st)
        STt = self.tmp("sgS", [128, 8], F32, stack=st)
        YB = self.tmp("sgY", [128, 256], F32, stack=st)
        for t in range(cfg.NT):
            ps = self.bank()
            self.proj_tok(t, W, 0, 512, ps[:, 0:512], ps)
            U, V, VT, VN, ST, Y = Ub.next(), Vg.next(), Vt.next(), VNb.next(), STt.next(), YB.next()
            self.act(U[:], ps[:, 0:256], AF.Gelu, reads=[ps], writes=[U])
            self.act(V[:], ps[:, 256:512], AF.Gelu, reads=[ps], writes=[V])
            self.layernorm(V[:], VN[:], self.psm("sgu_g"), self.psm("sgu_b"), 256, LN_EPS, reads=[V], preads=[self.PSM],
                           writes=[VN], st=ST, tmpn=VT)
            ps2 = self.bank()
            for g in range(4):
                self.mm(ps2[:, g * 64:(g + 1) * 64], WS[:, g, :], VN[:, g * 64:(g + 1) * 64], True, True,
                        reads=[WS, VN], writes=[ps2])
            self.tt(Y[:], ps2[:, 0:256], self.psm("sgu_bs"), ALU.add, reads=[ps2, self.PSM], writes=[Y])
            self.tt(Y[:], Y[:], U[:], ALU.mult, reads=[Y, U], writes=[Y])
            self.emit_yT(Y, YTm, t)

    def emit_yT(self, Y, YTm, t):
        ps3 = self.bank()
        for c in range(2):
            self.tr(ps3[:, c * 128:(c + 1) * 128], Y[:, c * 128:(c + 1) * 128], reads=[Y], writes=[ps3])
        self.cp(YTm[:, :, t * 128:(t + 1) * 128], ps3[:, 0:256].rearrange("p (a b) -> p a b", a=2), reads=[ps3],
                writes=[YTm], eng="act")

    def mix_ssd(self, s, l, st, YTm):
        cfg = self.cfg
        nc = self.nc
        W = self.alloc("Wssd", [128, KD, 772], BF16, stack=st)
        self.loadw(W[:], self.win_ap(l, O_CZ, O_CZ + 772), writes=[W], stream="win")
        AN = self.alloc("sdAN", [128, 4], F32, stack=st)
        self.act(AN[:], self.psm("a_log"), AF.Exp, reads=[self.PSM], writes=[AN])
        self.ts(AN[:], AN[:], -1.0, ALU.mult, reads=[AN], writes=[AN])
        XBC = self.alloc("sdXBC", [128, 4, 515], F32, stack=st)
        self.memset(XBC[:], 0.0, writes=[XBC], eng="pool")
        CAr = self.tmp("sdCA", [128, 512], F32, stack=st)
        XST = self.alloc("sdXST", [128, 2, 512], F32, stack=st)
        BTf = self.alloc("sdBTf", [128, 512], F32, stack=st)
        BTb = self.alloc("sdBTb", [128, 2, 512], BF16, stack=st)
        self.memset(BTb[:], 0.0, writes=[BTb], eng="pool")
        CTf = self.alloc("sdCTf", [128, 512], F32, stack=st)
        CTb = self.alloc("sdCTb", [128, 512], BF16, stack=st)
        S32 = self.alloc("sdS32", [128, 128], F32, stack=st)
        Sb = self.alloc("sdSb", [128, 128], BF16, stack=st)
        self.memset(S32[:], 0.0, writes=[S32])
        self.memset(Sb[:], 0.0, writes=[Sb])
        SZr = self.tmp("sdSZ", [128, 256], F32, stack=st)
        DTr = self.tmp("sdDT", [128, 24], F32, stack=st)
        for _b in DTr.bufs:
            self.memset(_b[:], 0.0, writes=[_b])
        XSr = self.tmp("sdXS", [128, 256], F32, stack=st)
        BKr = self.tmp("sdBK", [128, 128], BF16, stack=st)
        XDr = self.tmp("sdXD", [128, 256], BF16, stack=st)
        XDSr = self.tmp("sdXDS", [128, 256], BF16, stack=st)
        DABr = self.tmp("sdDAB", [128, 512], F32, n=1, stack=st)
        EARr = self.tmp("sdEAR", [128, 512], F32, stack=st)
        DFr = self.tmp("sdDF", [128, 512], F32, n=1, stack=st)
        WTr = self.tmp("sdWT", [128, 512], BF16, stack=st)
        CSTr = self.tmp("sdCST", [128, 512], BF16, stack=st)
        for _b in CSTr.bufs:
            self.memset(_b[:], 0.0, writes=[_b], eng="pool")
        Y1r = self.tmp("sdY1", [128, 256], F32, n=1, stack=st)
        SQr = self.tmp("sdSQ", [128, 256], F32, n=1, stack=st)
        STr = self.tmp("sdST", [128, 8], F32, stack=st)
        Yr = self.tmp("sdY", [128, 256], F32, stack=st)
        triu = self.cst("triu")
        negmask = self.cst("negmask")
        for g in range(cfg.NG):
            if g > 0:
                self.cp(XBC[:, :, 0:3], XBC[:, :, 512:515], reads=[XBC], writes=[XBC], eng="pool")
            for ch in range(4):
                ps = self.bank()
                self.proj_feat(g, W, 256 + ch * 128, 128, ps[:, :], ps)
                self.cp(XBC[:, ch, 3:515], ps[:, :], reads=[ps], writes=[XBC], eng=("act" if ch % 2 == 0 else "dve"))
            for ch in range(4):
                CA = CAr.next()
                eng = "dve"
                wc = lambda w: self.PCOL[:, 1 + ch * 4 + w:2 + ch * 4 + w]
                self.ts(CA[:], XBC[:, ch, 0:512], wc(0), ALU.mult, reads=[XBC, self.PCOL], writes=[CA], eng=eng)
                for w in range(1, 4):
                    self.stt(CA[:], XBC[:, ch, w:w + 512], wc(w), CA[:], ALU.mult, ALU.add, reads=[XBC, self.PCOL, CA], writes=[CA], eng=eng)
                dst, dbuf = [(XST[:, 0, :], XST), (XST[:, 1, :], XST), (BTf[:], BTf), (CTf[:], CTf)][ch]
                self.act(dst, CA[:], AF.Silu, reads=[CA, self.PCOL], writes=[dbuf], bias=self.PCOL[:, 17 + ch:18 + ch], scale=1.0)
            self.cp(BTb[0:64, 0, :], BTf[0:64, :], reads=[BTf], writes=[BTb], eng="pool")
            self.cp(BTb[64:128, 1, :], BTf[64:128, :], reads=[BTf], writes=[BTb], eng="pool")
            self.cp(CTb[:], CTf[:], reads=[CTf], writes=[CTb], eng="pool")
            import os
            L3 = float(os.environ.get("STOP3", "99"))
            for c in range(4):
                if L3 < 1:
                    continue
                t = 4 * g + c
                sl = slice(c * 128, (c + 1) * 128)
                psz = self.bank()
                self.proj_tok(t, W, 0, 256, psz[:, 0:256], psz)
                self.proj_tok(t, W, 768, 4, psz[:, 256:260], psz)
                SZ, DT = SZr.next(), DTr.next()
                self.act(SZ[:], psz[:, 0:256], AF.Silu, reads=[psz], writes=[SZ])
                self.tt(DT[:, 0:4], psz[:, 256:260], self.psm("dt_bias"), ALU.add, reads=[psz, self.PSM], writes=[DT])
                self.act(DT[:, 0:4], DT[:, 0:4], AF.Exp, reads=[DT], writes=[DT])
                self.act(DT[:, 4:8], DT[:, 0:4], AF.Ln, reads=[DT], writes=[DT], bias=1.0)
                self.tt(DT[:, 8:12], DT[:, 4:8], AN[:], ALU.mult, reads=[DT, AN], writes=[DT])
                if L3 < 2:
                    continue
                pst = self.bank()
                for j in range(2):
                    self.tr(pst[:, j * 128:(j + 1) * 128], XST[:, j, sl], reads=[XST], writes=[pst])
                self.tr(pst[:, 256:384], BTf[:, sl], reads=[BTf], writes=[pst])
                XS, BK, XD, XDS = XSr.next(), BKr.next(), XDr.next(), XDSr.next()
                self.cp(XS[:], pst[:, 0:256], reads=[pst], writes=[XS], eng="act")
                self.cp(BK[:], pst[:, 256:384], reads=[pst], writes=[BK], eng="dve")
                xs4 = XS[:, :].rearrange("p (h v) -> p h v", h=4)
                self.tt(XD[:, :].rearrange("p (h v) -> p h v", h=4), xs4, DT[:, 4:8].unsqueeze(2).to_broadcast([128, 4, 64]),
                        ALU.mult, reads=[XS, DT], writes=[XD])
                if L3 < 3:
                    continue
                DAB = DABr.next()
                self.cp(DAB[:, :].rearrange("p (h i) -> p h i", h=4), DT[:, 8:12].unsqueeze(2).to_broadcast([128, 4, 128]),
                        reads=[DT], writes=[DAB], eng="dve")
                psr, psc = self.bank(), self.bank()
                for h in range(4):
                    self.mm(psr[:, h * 128:(h + 1) * 128], DAB[:, h * 128:(h + 1) * 128], triu, True, True,
                            reads=[DAB, self.CONST], writes=[psr])
                if L3 < 3.2:
                    continue
                self.mm(psc[:, 0:16], triu, DT[:, 8:24], True, True, reads=[DT, self.CONST], writes=[psc])
                self.cp(DT[:, 12:16], psc[:, 0:4], reads=[psc], writes=[DT], eng="dve")
                if L3 < 3.4:
                    continue
                EAR, DF = EARr.next(), DFr.next()
                self.act(EAR[:], psr[:, :], AF.Exp, reads=[psr], writes=[EAR])
                if L3 < 3.6:
                    continue
                for h in range(4):
                    self.stt(DF[:, h * 128:(h + 1) * 128], psr[:, h * 128:(h + 1) * 128], DT[:, 12 + h:13 + h], negmask,
                             ALU.subtract, ALU.add, reads=[psr, DT, self.CONST], writes=[DF])
                if L3 < 3.8:
                    continue
                self.act(DAB[:], DF[:], AF.Exp, reads=[DF], writes=[DAB])
                if L3 < 4:
                    continue
                psg = self.bank()
                for gi in range(2):
                    self.mm(psg[:, gi * 128:(gi + 1) * 128], BTb[:, gi, sl], CTb[:, sl], True, True, reads=[BTb, CTb], writes=[psg])
                if L3 < 4.2:
                    continue
                WT = WTr.next()
                for h in range(4):
                    gi = h // 2
                    self.tt(WT[:, h * 128:(h + 1) * 128], DAB[:, h * 128:(h + 1) * 128], psg[:, gi * 128:(gi + 1) * 128], ALU.mult,
                            reads=[DAB, psg], writes=[WT])
                if L3 < 4.4:
                    continue
                CST = CSTr.next()
                for h in range(4):
                    gi, hh = h // 2, h % 2
                    r_ = slice(64 * gi, 64 * gi + 64)
                    self.tt(CST[r_, h * 128:(h + 1) * 128], CTf[r_, sl], EAR[r_, h * 128:(h + 1) * 128], ALU.mult,
                            reads=[CTf, EAR], writes=[CST])
                if L3 < 5:
                    continue
                psy = self.bank()
                for h in range(4):
                    gi, hh = h // 2, h % 2
                    r_ = slice(64 * gi, 64 * gi + 64)
                    self.mm(psy[:, h * 64:(h + 1) * 64], WT[:, h * 128:(h + 1) * 128], XD[:, h * 64:(h + 1) * 64], True, False,
                            reads=[WT, XD], writes=[psy])
                    self.mm(psy[:, h * 64:(h + 1) * 64], CST[:, h * 128:(h + 1) * 128], Sb[:, hh * 64:(hh + 1) * 64], False, True,
                            reads=[CST, Sb], writes=[psy])
                if L3 < 6:
                    continue
                self.tt(DT[:, 16:20].unsqueeze(2), psr[:, :].rearrange("p (h i) -> p h i", h=4)[:, :, 127:128], DT[:, 12:16].unsqueeze(2),
                        ALU.subtract, reads=[psr, DT], writes=[DT])
                self.act(DT[:, 16:20], DT[:, 16:20], AF.Exp, reads=[DT], writes=[DT])
                self.tt(DT[:, 20:24], DT[:, 16:20], DT[:, 4:8], ALU.mult, reads=[DT], writes=[DT])
                self.tt(XDS[:, :].rearrange("p (h v) -> p h v", h=4), xs4, DT[:, 20:24].unsqueeze(2).to_broadcast([128, 4, 64]),
                        ALU.mult, reads=[XS, DT], writes=[XDS])
                pss = self.bank()
                self.mm(pss[:, 0:256], BK[:], XDS[:], True, True, reads=[BK, XDS], writes=[pss])
                for h in range(4):
                    gi, hh = h // 2, h % 2
                    r_ = slice(64 * gi, 64 * gi + 64)
                    self.stt(S32[r_, hh * 64:(hh + 1) * 64], S32[r_, hh * 64:(hh + 1) * 64], EAR[r_, h * 128 + 127:h * 128 + 128],
                             pss[r_, h * 64:(h + 1) * 64], ALU.mult, ALU.add, reads=[S32, EAR, pss], writes=[S32])
                self.cp(Sb[:], S32[:], reads=[S32], writes=[Sb], eng="pool")
                if L3 < 7:
                    continue
                Y1, SQ, ST, Y = Y1r.next(), SQr.next(), STr.next(), Yr.next()
                self.tt(Y1[:], XS[:], self.psm("ssm_d"), ALU.mult, reads=[XS, self.PSM], writes=[Y1], eng="pool")
                self.tt(Y1[:], Y1[:], psy[:, 0:256], ALU.add, reads=[Y1, psy], writes=[Y1])
                self.tt(Y1[:], Y1[:], SZ[:], ALU.mult, reads=[Y1, SZ], writes=[Y1])
                self.tt(SQ[:], Y1[:], Y1[:], ALU.mult, reads=[Y1], writes=[SQ], eng="pool")
                self.S.op("dve", lambda: nc.vector.tensor_reduce(out=ST[:, 0:2], in_=SQ[:, :].rearrange("p (g v) -> p g v", g=2),
                                                                 axis=AX.X, op=ALU.add), reads=[SQ], writes=[ST])
                self.act(ST[:, 0:2], ST[:, 0:2], AF.Sqrt, reads=[ST], writes=[ST], scale=1.0 / 128, bias=float(RMS_EPS))
                self.S.op("dve", lambda: nc.vector.reciprocal(out=ST[:, 4:6], in_=ST[:, 0:2]), reads=[ST], writes=[ST])
                self.tt(Y[:, :].rearrange("p (g v) -> p g v", g=2), Y1[:, :].rearrange("p (g v) -> p g v", g=2),
                        ST[:, 4:6].unsqueeze(2).to_broadcast([128, 2, 128]), ALU.mult, reads=[Y1, ST], writes=[Y])
                self.tt(Y[:], Y[:], self.psm("ssm_nw"), ALU.mult, reads=[Y, self.PSM], writes=[Y])
                self.emit_yT(Y, YTm, t)

    def rope_tables(self, s):
        cfg = self.cfg
        nc = self.nc
        NT = cfg.NT
        if not hasattr(self, "COS"):
            self.COS = self.alloc("COS", [128, NT, 8], F32)
            self.SIN = self.alloc("SIN", [128, NT, 8], F32)
        with ExitStack() as st:
            PI_ = self.alloc("rpI", [128, NT], I32, stack=st)
            PF = self.alloc("rpF", [128, NT], F32, stack=st)
            ANG = self.alloc("rpA", [128, NT, 8], F32, stack=st)
            TQ = self.alloc("rpQ", [128, NT, 8], F32, stack=st)
            KI = self.alloc("rpK", [128, NT, 8], I32, stack=st)
            KF = self.alloc("rpKF", [128, NT, 8], F32, stack=st)
            self.load(PI_[:], self.d_pos[s * 128:(s + 1) * 128, :], writes=[PI_])
            self.cp(PF[:], PI_[:], reads=[PI_], writes=[PF])
            self.tt(ANG[:], PF[:].unsqueeze(2).to_broadcast([128, NT, 8]), self.cst("invf").unsqueeze(1).to_broadcast([128, NT, 8]),
                    ALU.mult, reads=[PF, self.CONST], writes=[ANG])
            C1 = 6.28125
            C2 = 2.0 * math.pi - C1
            for dst, shift in ((self.SIN, 0.0), (self.COS, math.pi / 2)):
                src = ANG
                if shift != 0.0:
                    self.ts(TQ[:], ANG[:], shift, ALU.add, reads=[ANG], writes=[TQ])
                    src = TQ
                self.ts(KF[:], src[:], 1.0 / (2.0 * math.pi), ALU.mult, reads=[src], writes=[KF])
                self.cp(KI[:], KF[:], reads=[KF], writes=[KI])
                self.cp(KF[:], KI[:], reads=[KI], writes=[KF])
                self.stt(dst[:], KF[:], -C1, src[:], ALU.mult, ALU.add, reads=[KF, src], writes=[dst])
                self.stt(dst[:], KF[:], -C2, dst[:], ALU.mult, ALU.add, reads=[KF, dst], writes=[dst])
                self.ts(dst[:], dst[:], math.pi, ALU.min, reads=[dst], writes=[dst], s2=-math.pi, op1=ALU.max)
                self.act(dst[:], dst[:], AF.Sin, reads=[dst], writes=[dst])
            self.barrier()

    def mix_dil(self, s, l, st, YTm):
        cfg = self.cfg
        nc = self.nc
        T, NT = cfg.T, cfg.NT
        W = self.alloc("Wdil", [128, KD, 384], BF16, stack=st)
        QKT = self.alloc("dlQKT", [128, 2, T], BF16, stack=st)
        VP = [self.alloc(f"dlVP{i}", [128, NT, 128], BF16, stack=st) for i in range(3)]
        ACD = self.alloc("dlACD", [128, 2, T], F32, stack=st)
        NEGC = self.alloc("dlNEGC", [33, T], BF16, stack=st)
        ONESB = self.alloc("dlONES", [128, 128], BF16, stack=st)
        self.memset(ONESB[:], 1.0, writes=[ONESB])
        SEL = self.alloc("dlSEL", [128, 33], BF16, stack=st)
        self.memset(SEL[:], 0.0, writes=[SEL])
        self.memset(SEL[0:64, 0:1], 1.0, writes=[SEL])
        self.memset(SEL[64:128, 32:33], 1.0, writes=[SEL])
        KM = self.alloc("dlKM", [33, 8], F32, stack=st)
        RQr = self.tmp("dlRQ", [128, 256], F32, stack=st)
        T1r = self.tmp("dlT1", [128, 64], F32, stack=st)
        SQr = self.tmp("dlSQ", [128, 512], BF16, stack=st)
        C2r = self.tmp("dlC2", [33, 512], F32, stack=st)
        PTr = self.tmp("dlPT", [128, 256], BF16, n=4, stack=st)
        RD = self.alloc("dlRD", [128, 512], F32, stack=st)
        allXT = list(self.XT)
        dils = (1, 4, 16)
        for pair in range(2):
            for j in range(3):
                c0 = O_DQ + j * 256 + pair * 128
                self.loadw(W[:, :, j * 128:(j + 1) * 128], self.win_ap(l, c0, c0 + 128), writes=[W], stream="win")
            for t in range(NT):
                ps = self.bank()
                self.proj_tok(t, W, 0, 256, ps[:, 0:256], ps)
                RQ, T1 = RQr.next(), T1r.next()
                self.cp(RQ[:], ps[:, 0:256], reads=[ps], writes=[RQ], eng="act")
                pv = ps[:, 0:256].rearrange("p (h d) -> p h d", h=4)
                rv = RQ[:, :].rearrange("p (h d) -> p h d", h=4)
                cb = self.COS[:, t, :].unsqueeze(1).to_broadcast([128, 4, 8])
                sb = self.SIN[:, t, :].unsqueeze(1).to_broadcast([128, 4, 8])
                ta = T1[:, 0:32].rearrange("p (h d) -> p h d", h=4)
                tb = T1[:, 32:64].rearrange("p (h d) -> p h d", h=4)
                self.tt(ta, pv[:, :, 0:8], cb, ALU.mult, reads=[ps, self.COS], writes=[T1])
                self.tt(tb, pv[:, :, 8:16], sb, ALU.mult, reads=[ps, self.SIN], writes=[T1])
                self.tt(rv[:, :, 0:8], ta, tb, ALU.subtract, reads=[T1], writes=[RQ])
                self.tt(ta, pv[:, :, 8:16], cb, ALU.mult, reads=[ps, self.COS], writes=[T1])
                self.tt(tb, pv[:, :, 0:8], sb, ALU.mult, reads=[ps, self.SIN], writes=[T1])
                self.tt(rv[:, :, 8:16], ta, tb, ALU.add, reads=[T1], writes=[RQ])
                ps2 = self.bank()
                for j in range(2):
                    self.tr(ps2[:, j * 128:(j + 1) * 128], RQ[:, j * 128:(j + 1) * 128], reads=[RQ], writes=[ps2])
                self.cp(QKT[:, :, t * 128:(t + 1) * 128], ps2[:, 0:256].rearrange("p (a b) -> p a b", a=2), reads=[ps2], writes=[QKT],
                        eng="dve")
            for which in (1, 0):
                for g in range(cfg.NG):
                    SQ = SQr.next()
                    src = QKT[:, which, g * 512:(g + 1) * 512]
                    self.tt(SQ[:], src, src, ALU.mult, reads=[QKT], writes=[SQ], eng="pool")
                    psn = self.bank()
                    self.mm(psn[0:33, :], SEL[:], SQ[:], True, True, reads=[SEL, SQ], writes=[psn])
                    if which == 1:
                        self.S.op("dve", lambda: nc.vector.reduce_max(out=KM[:, g:g + 1], in_=psn[0:33, :], axis=AX.X), reads=[psn], writes=[KM])
                    else:
                        C2 = C2r.next()
                        self.ts(C2[:], psn[0:33, :], KM[:, 4:5], ALU.mult, reads=[psn, KM], writes=[C2])
                        self.act(C2[:], C2[:], AF.Sqrt, reads=[C2], writes=[C2])
                        self.ts(NEGC[:, g * 512:(g + 1) * 512], C2[:], -1.0, ALU.mult, reads=[C2], writes=[NEGC])
                if which == 1:
                    self.S.op("dve", lambda: nc.vector.reduce_max(out=KM[:, 4:5], in_=KM[:, 0:cfg.NG], axis=AX.X), reads=[KM], writes=[KM])
            for bi, dil in enumerate(dils):
                nb = NT // dil
                for r in range(dil):
                    for b in range(nb):
                        blk = r * nb + b
                        ps = self.bank()
                        for k in range(KD):
                            lhs = self.XTh[:, k, :].rearrange("p (l r) -> p r l", r=dil)[:, r, b * 128:(b + 1) * 128]
                            self.mm(ps[:, 0:128], lhs, W[:, k, 256:384], k == 0, k == KD - 1,
                                    reads=allXT + [W], writes=[ps])
                        self.cp(VP[bi][:, blk, :], ps[:, 0:128], reads=[ps], writes=[VP[bi]], eng=("act" if blk % 2 == 0 else "dve"))
            for bi, dil in enumerate(dils):
                nb = NT // dil
                qv = QKT[:, 0, :].rearrange("p (l r) -> p r l", r=dil)
                kv = QKT[:, 1, :].rearrange("p (l r) -> p r l", r=dil)
                cv = NEGC[:, :].rearrange("p (l r) -> p r l", r=dil)
                av = ACD[:, :, :].rearrange("p a (l r) -> p a r l", r=dil)
                for r in range(dil):
                    Bq = None
                    for b in range(nb):
                        blk = r * nb + b
                        nq = 256 if b + 1 < nb else 128
                        PTs = []
                        for hh in range(2):
                            R_ = slice(64 * hh, 64 * hh + 64)
                            sr = 32 * hh
                            pss = self.bank()
                            self.mm(pss[:, 0:nq], kv[R_, r, b * 128:(b + 1) * 128], qv[R_, r, b * 128:b * 128 + nq], True, False,
                                    reads=[QKT], writes=[pss])
                            self.mm(pss[:, 0:nq], self.IDB[:], self.DMASK[:, 0:nq], False, False, reads=[self.IDB, self.DMASK], writes=[pss])
                            self.mm(pss[:, 0:nq], ONESB[sr:sr + 1, :], cv[sr:sr + 1, r, b * 128:b * 128 + nq], False, True,
                                    reads=[ONESB, NEGC], writes=[pss])
                            PT = PTr.next()
                            self.act(PT[:, 0:nq], pss[:, 0:nq], AF.Exp, reads=[pss], writes=[PT], scale=0.125)
                            PTs.append(PT)
                        if Bq is None:
                            Bq = (self.bank(), self.bank())
                        for hh in range(2):
                            R_ = slice(64 * hh, 64 * hh + 64)
                            self.mm(Bq[0][R_, 0:128], VP[bi][:, blk, R_], PTs[hh][:, 0:128], b == 0, True, reads=[VP[bi], PTs[hh]], writes=[Bq[0]])
                            self.mm(Bq[1][R_, 0:128], ONESB[:, 0:64], PTs[hh][:, 0:128], b == 0, True, reads=[ONESB, PTs[hh]], writes=[Bq[1]])
                        for a in range(2):
                            dst = av[:, a, r, b * 128:(b + 1) * 128]
                            src = Bq[a][:, 0:128]
                            if bi == 0:
                                self.cp(dst, src, reads=[Bq[a]], writes=[ACD], eng=("dve" if a == 0 else "act"))
                            else:
                                self.tt(dst, src, dst, ALU.add, reads=[Bq[a], ACD], writes=[ACD])
                        Bq = None
                        if b + 1 < nb:
                            Bq = (self.bank(), self.bank())
                            for hh in range(2):
                                R_ = slice(64 * hh, 64 * hh + 64)
                                self.mm(Bq[0][R_, 0:128], VP[bi][:, blk, R_], PTs[hh][:, 128:256], True, False, reads=[VP[bi], PTs[hh]], writes=[Bq[0]])
                                self.mm(Bq[1][R_, 0:128], ONESB[:, 0:64], PTs[hh][:, 128:256], True, False, reads=[ONESB, PTs[hh]], writes=[Bq[1]])
            for g in range(cfg.NG):
                sl = slice(g * 512, (g + 1) * 512)
                self.S.op("dve", lambda: nc.vector.reciprocal(out=RD[:], in_=ACD[:, 1, sl]), reads=[ACD], writes=[RD])
                self.tt(YTm[:, pair, sl], ACD[:, 0, sl], RD[:], ALU.mult, reads=[ACD, RD], writes=[YTm])

    def outproj_ln_router(self, s, l, st):
        cfg = self.cfg
        nc = self.nc
        WO = self.alloc("WO", [128, KD, D], BF16, stack=st)
        self.loadw(WO[:], self.d_wout[l * D:(l + 1) * D, :].rearrange("(k p) n -> p k n", p=128), writes=[WO], stream="win")
        RW = self.alloc("RW", [128, KD, NE], F32, stack=st)
        self.load(RW[:], self.d_rw[l * D:(l + 1) * D, :].rearrange("(k p) n -> p k n", p=128), writes=[RW])
        LNP = self.alloc("LNP", [128, 2048], F32, stack=st)
        self.load(LNP[:], self.d_pvec[l * 128:(l + 1) * 128, 0:2048], writes=[LNP])
        Tt = self.tmp("Tt", [128, D], F32, stack=st)
        Tn = self.tmp("Tn", [128, D], F32, stack=st)
        STt = self.tmp("lnS", [128, 8], F32, stack=st)
        HTf = self.tmp("HTf", [128, KD, 128], F32, stack=st)
        R = {k: self.tmp("rt" + k, [128, w], F32, stack=st) for k, w in
             [("sc", 128), ("ch", 128), ("m8", 64), ("gs", 8), ("m8b", 8), ("gm", 8), ("pen", 8), ("cm", 128),
              ("m8c", 8), ("sel", 128), ("gu", 128), ("sm", 2)]}
        for t in range(cfg.NT):
            psA, psB = self.bank(), self.bank()
            for half, ps in enumerate((psA, psB)):
                for k in range(KD):
                    m = k // 2
                    self.mm(ps[:, :], self.YTh[:, k, t * 128:(t + 1) * 128], WO[:, k, half * 512:(half + 1) * 512],
                            k == 0, k == KD - 1, reads=[self.YT[m], WO], writes=[ps])
            T_, TN, ST = Tt.next(), Tn.next(), STt.next()
            for half, ps in enumerate((psA, psB)):
                sl = slice(half * 512, (half + 1) * 512)
                self.stt(T_[:, sl], self.X[t][:, sl], ALPHA, ps[:, :], ALU.mult, ALU.add, reads=[self.X[t], ps], writes=[T_])
            self.layernorm(T_[:], self.X[t][:], LNP[:, 0:1024], LNP[:, 1024:2048], D, LN_EPS, reads=[T_], preads=[LNP],
                           writes=[self.X[t]], st=ST, tmpn=TN)
            if cfg.dbg and s == 0 and l == 0:
                self.load(self.dbg["dbg_h"][t * 128:(t + 1) * 128, :], self.X[t][:], writes=[], reads=[self.X[t]], stream="dbg")
            import os
            lvl = int(os.environ.get("STOP2", "9"))
            if lvl < 1:
                continue
            H = HTf.next()
            self.transpose_tile(t, htf=H)
            if lvl < 2:
                continue
            psR = self.bank()
            for k in range(KD):
                self.mm(psR[:, 0:NE], H[:, k, :], RW[:, k, :], k == 0, k == KD - 1, reads=[H, RW], writes=[psR])
            if lvl < 3:
                continue
            self.routing(t, psR, R)
            if lvl < 4:
                continue
            if cfg.dbg and s == 0 and l == 0:
                self.load(self.dbg["dbg_G"][t * 128:(t + 1) * 128, :], self.Gh[:, t, 0:NE], writes=[], reads=[self.G], stream="dbg")
            self.ts(self.X[t][:], self.X[t][:], ALPHA, ALU.mult, reads=[self.X[t]], writes=[self.X[t]], eng="pool")

    def routing(self, t, psR, R):
        nc = self.nc
        sc, ch, m8, gs, m8b, gm, pen, cm, m8c, sel, gu, sm = [R[k].next() for k in
                                                               ("sc", "ch", "m8", "gs", "m8b", "gm", "pen", "cm", "m8c", "sel", "gu", "sm")]
        self.act(sc[:], psR[:, 0:NE], AF.Sigmoid, reads=[psR], writes=[sc])
        self.tt(ch[:], sc[:], self.psm("r_bias"), ALU.add, reads=[sc, self.PSM], writes=[ch])
        for g in range(8):
            self.S.op("dve", lambda: nc.vector.max(out=m8[:, g * 8:(g + 1) * 8], in_=ch[:, g * 16:(g + 1) * 16]), reads=[ch], writes=[m8])
        m8v = m8[:, :].rearrange("p (g k) -> p g k", g=8)
        self.tt(gs[:].unsqueeze(2), m8v[:, :, 0:1], m8v[:, :, 1:2], ALU.add, reads=[m8], writes=[gs])
        self.S.op("dve", lambda: nc.vector.max(out=m8b[:], in_=gs[:]), reads=[gs], writes=[m8b])
        self.ts(gm[:], gs[:], m8b[:, 3:4], ALU.is_ge, reads=[gs, m8b], writes=[gm])
        self.ts(pen[:], gm[:], 4.0, ALU.mult, reads=[gm], writes=[pen], s2=-4.0, op1=ALU.add)
        chv = ch[:, :].rearrange("p (g k) -> p g k", g=8)
        cmv = cm[:, :].rearrange("p (g k) -> p g k", g=8)
        self.tt(cmv, chv, gm[:].unsqueeze(2).to_broadcast([128, 8, 16]), ALU.mult, reads=[ch, gm], writes=[cm])
        self.tt(cmv, cmv, pen[:].unsqueeze(2).to_broadcast([128, 8, 16]), ALU.add, reads=[cm, pen], writes=[cm])
        self.S.op("dve", lambda: nc.vector.max(out=m8c[:], in_=cm[:]), reads=[cm], writes=[m8c])
        self.ts(sel[:], cm[:], m8c[:, 7:8], ALU.is_ge, reads=[cm, m8c], writes=[sel])
        self.tt(gu[:], sc[:], sel[:], ALU.mult, reads=[sc, sel], writes=[gu])
        self.S.op("dve", lambda: nc.vector.reduce_sum(out=sm[:, 0:1], in_=gu[:], axis=AX.X), reads=[gu], writes=[sm])
        self.S.op("dve", lambda: nc.vector.reciprocal(out=sm[:, 1:2], in_=sm[:, 0:1]), reads=[sm], writes=[sm])
        self.ts(self.Gh[:, t, 0:NE], gu[:], sm[:, 1:2], ALU.mult, reads=[gu, sm], writes=[self.G])

    def exp_w_aps(self, l, e):
        if e < NE:
            r = (l * NE + e) * D
            wg = self.d_wg[r:r + D, :]
            wu = self.d_wu[r:r + D, :]
            r2 = (l * NE + e) * 256
            wd = self.d_wd[r2:r2 + 256, :]
        else:
            wg = self.d_swg[l * D:(l + 1) * D, :]
            wu = self.d_swu[l * D:(l + 1) * D, :]
            wd = self.d_swd[l * 256:(l + 1) * 256, :]
        return (wg.rearrange("(k p) n -> p k n", p=128), wu.rearrange("(k p) n -> p k n", p=128),
                wd.rearrange("(k p) n -> p k n", p=128))

    def moe(self, s, l, st):
        cfg = self.cfg
        nc = self.nc
        NB = 3
        WGU = [self.alloc(f"WGU{i}", [128, KD, 512], BF16, stack=st) for i in range(NB)]
        WD = [self.alloc(f"WD{i}", [128, 2, D], BF16, stack=st) for i in range(NB)]
        AT = [self.alloc(f"AT{i}", [128, 2, cfg.T], BF16, stack=st) for i in range(2)]
        SG = self.tmp("SG", [128, 512], F32, n=3, stack=st)
        elist = list(range(cfg.n_exp - 1)) + [NE]

        def issue(i):
            e = elist[i]
            wg, wu, wd = self.exp_w_aps(l, e)
            b = i % NB
            self.loadw(WGU[b][:, :, 0:256], wg, writes=[WGU[b]], stream=f"we{b}", rot=1, chain=False)
            self.loadw(WGU[b][:, :, 256:512], wu, writes=[WGU[b]], stream=f"we{b}", rot=1, chain=False)
            self.loadw(WD[b][:], wd, writes=[WD[b]], stream=f"we{b}", rot=1, chain=False)
        for i in range(min(NB - 1, len(elist))):
            issue(i)
        for i, e in enumerate(elist):
            if i + NB - 1 < len(elist):
                issue(i + NB - 1)
            b = i % NB
            A = AT[i % 2]
            for c in range(2):
                for g in range(cfg.NG):
                    psg, psu = self.bank(), self.bank()
                    self.proj_feat(g, WGU[b], c * 128, 128, psg[:, :], psg)
                    self.proj_feat(g, WGU[b], 256 + c * 128, 128, psu[:, :], psu)
                    sg = SG.next()
                    self.act(sg[:], psg[:, :], AF.Silu, reads=[psg], writes=[sg])
                    self.tt(A[:, c, g * 512:(g + 1) * 512], psu[:, :], sg[:], ALU.mult, reads=[psu, sg], writes=[A])
            for t in range(cfg.NT):
                py = (self.bank(), self.bank())
                for half in range(2):
                    for c in range(2):
                        self.mm(py[half][:, :], A[:, c, t * 128:(t + 1) * 128], WD[b][:, c, half * 512:(half + 1) * 512],
                                c == 0, c == 1, reads=[A, WD[b]], writes=[py[half]])
                for half in range(2):
                    sl = slice(half * 512, (half + 1) * 512)
                    self.stt(self.X[t][:, sl], py[half][:, :], self.Gh[:, t, e:e + 1], self.X[t][:, sl], ALU.mult, ALU.add,
                             reads=[py[half], self.G, self.X[t]], writes=[self.X[t]])
        LNP = self.alloc("LNP2", [128, 2048], F32, stack=st)
        self.load(LNP[:], self.d_pvec[l * 128:(l + 1) * 128, 2048:4096], writes=[LNP])
        Tn = self.tmp("Tn2", [128, D], F32, stack=st)
        STt = self.tmp("lnS2", [128, 8], F32, stack=st)
        last = (l == cfg.NL - 1)
        for t in range(cfg.NT):
            self.layernorm(self.X[t][:], self.X[t][:], LNP[:, 0:1024], LNP[:, 1024:2048], D, LN_EPS, reads=[self.X[t]],
                           preads=[LNP], writes=[self.X[t]], st=STt.next(), tmpn=Tn.next())
            if last:
                r0 = s * cfg.T + t * 128
                self.load(self.d_out[r0:r0 + 128, :], self.X[t][:], writes=[], reads=[self.X[t]], stream="out")
            else:
                self.transpose_tile(t)


def prep_core_inputs(inp, cfg, core):
    NL, T, NSEQ, NT = cfg.NL, cfg.T, cfg.NSEQ, cfg.NT
    f = lambda a: np.ascontiguousarray(np.asarray(a))
    seqs = [core * NSEQ + i for i in range(NSEQ)]
    x = f(inp["x"])[seqs][:, :T].reshape(NSEQ * T, D)
    pos = f(inp["positions"])[seqs][:, :T].reshape(NSEQ, NT, 128).transpose(0, 2, 1).reshape(NSEQ * 128, NT)
    m = {"x": x, "pos": np.ascontiguousarray(pos).astype(np.int32)}
    m["w_in"] = f(inp["w_in"])[:NL].reshape(NL * D, NIN)
    m["w_out"] = f(inp["w_out"])[:NL].reshape(NL * D, D)
    m["router_w"] = f(inp["router_w"])[:NL].reshape(NL * D, NE)
    import os
    if os.environ.get("STOP", "") in ("mix", "load"):
        m["exp_w_gate"] = np.zeros((8, 256), np.float32); m["exp_w_up"] = np.zeros((8, 256), np.float32)
        m["exp_w_down"] = np.zeros((8, D), np.float32)
    else:
        m["exp_w_gate"] = f(inp["exp_w_gate"])[:NL].reshape(NL * NE * D, 256)
        m["exp_w_up"] = f(inp["exp_w_up"])[:NL].reshape(NL * NE * D, 256)
        m["exp_w_down"] = f(inp["exp_w_down"])[:NL].reshape(NL * NE * 256, D)
    m["sh_w_gate"] = f(inp["sh_w_gate"])[:NL].reshape(NL * D, 256)
    m["sh_w_up"] = f(inp["sh_w_up"])[:NL].reshape(NL * D, 256)
    m["sh_w_down"] = f(inp["sh_w_down"])[:NL].reshape(NL * 256, D)
    m["gla_w_gate"] = f(inp["gla_w_gate"])[:NL].reshape(NL * 16, 128)
    m["sgu_wT"] = np.ascontiguousarray(f(inp["sgu_w"])[:NL].transpose(0, 1, 3, 2)).reshape(NL * 4 * 128, 128)
    pv = np.zeros((NL, 128, NPV), np.float32)

    def put(name, arr):
        o, w = PV[name]
        pv[:, :, o:o + w] = arr[:, None, :]
    put("ln1_g", f(inp["ln1_g"])[:NL]); put("ln1_b", f(inp["ln1_b"])[:NL])
    put("ln2_g", f(inp["ln2_g"])[:NL]); put("ln2_b", f(inp["ln2_b"])[:NL])
    put("sgu_g", f(inp["sgu_ln_g"])[:NL]); put("sgu_b", f(inp["sgu_ln_b"])[:NL])
    put("gla_nw", np.tile(f(inp["gla_norm_w"])[:NL], (1, 4)))
    put("ssm_nw", f(inp["ssm_norm_w"])[:NL])
    put("ssm_d", np.repeat(f(inp["ssm_d"])[:NL], 64, axis=1))
    put("dt_bias", f(inp["ssm_dt_bias"])[:NL]); put("a_log", f(inp["ssm_a_log"])[:NL])
    put("r_bias", f(inp["router_bias"])[:NL])
    o, w = PV["sgu_bs"]
    sb = f(inp["sgu_b"])[:NL]
    pv[:, :, o:o + w] = np.repeat(sb.transpose(0, 2, 1), 64, axis=2)
    m["pvec"] = pv.reshape(NL * 128, NPV)
    pc = np.zeros((NL, 128, NPC), np.float32)
    pc[:, :, 0] = f(inp["gla_b_gate"])[:NL]
    cw = f(inp["ssm_conv_w"])[:NL]
    pc[:, :, 1:17] = cw.reshape(NL, 4, 4, 128).transpose(0, 3, 2, 1).reshape(NL, 128, 16)
    cb = f(inp["ssm_conv_b"])[:NL]
    pc[:, :, 17:21] = cb.reshape(NL, 4, 128).transpose(0, 2, 1)
    m["pcol"] = pc.reshape(NL * 128, NPC)
    m["consts"] = make_consts()
    return m


_NC_CACHE = {}


def kernel(**inputs):
    from concourse.bass_utils import run_bass_kernel_spmd
    n_cores = 8
    cfg = Cfg(T=2048, NSEQ=2, NL=4, n_exp=129)
    if "nc" not in _NC_CACHE:
        _NC_CACHE["nc"] = MK(cfg).build()
    nc = _NC_CACHE["nc"]
    inp = {k: np.asarray(v) for k, v in inputs.items()}
    shared = None
    in_maps = []
    for c in range(n_cores):
        if shared is None:
            m = prep_core_inputs(inp, cfg, c)
            shared = m
        else:
            m = dict(shared)
            seqs = [c * cfg.NSEQ + i for i in range(cfg.NSEQ)]
            m["x"] = np.ascontiguousarray(inp["x"][seqs].reshape(cfg.NSEQ * cfg.T, D))
            pos = inp["positions"][seqs].reshape(cfg.NSEQ, cfg.NT, 128).transpose(0, 2, 1).reshape(cfg.NSEQ * 128, cfg.NT)
            m["pos"] = np.ascontiguousarray(pos).astype(np.int32)
        in_maps.append(m)
    res = run_bass_kernel_spmd(nc, in_maps, core_ids=list(range(n_cores)))
    outs = [np.asarray(r["out"]).reshape(cfg.NSEQ, cfg.T, D) for r in res.results]
    return np.concatenate(outs, axis=0).astype(np.float32)
```

```python
import math
import numpy as np
from contextlib import ExitStack
import concourse.bass as bass
import concourse.mybir as mybir

F32 = mybir.dt.float32
BF16 = mybir.dt.bfloat16
I32 = mybir.dt.int32
AF = mybir.ActivationFunctionType
ALU = mybir.AluOpType
AX = mybir.AxisListType

D = 1024
KD = 8
NIN = 2836
NE = 128
DEPTH = 4
ALPHA = (2 * DEPTH) ** 0.25
LN_EPS = 1e-5
RMS_EPS = 1e-6
O_AQ, O_AK, O_AV, O_AR, O_ALR = 0, 128, 256, 512, 768
O_BU, O_BV = 784, 1040
O_CZ, O_CXBC, O_CDT = 1296, 1552, 2064
O_DQ, O_DK, O_DV = 2068, 2324, 2580

PV = {}
_o = 0
for _n, _w in [("ln1_g", 1024), ("ln1_b", 1024), ("ln2_g", 1024), ("ln2_b", 1024),
               ("sgu_g", 256), ("sgu_b", 256), ("gla_nw", 256), ("ssm_nw", 256), ("ssm_d", 256),
               ("dt_bias", 4), ("a_log", 4), ("r_bias", 128), ("sgu_bs", 256)]:
    PV[_n] = (_o, _w)
    _o += _w
NPV = _o
PSM0 = PV["sgu_g"][0]
NPSM = NPV - PSM0
NPC = 1 + 16 + 4
CO = {}
_o = 0
for _n, _w in [("ident", 128), ("triu", 128), ("negmask", 128), ("dilmask", 256), ("invf", 8), ("selden", 64)]:
    CO[_n] = (_o, _w)
    _o += _w
NCONST = _o


def make_consts():
    c = np.zeros((128, NCONST), np.float32)
    p = np.arange(128)[:, None]
    f = np.arange(128)[None, :]
    c[:, CO["ident"][0]:CO["ident"][0] + 128] = (p == f)
    c[:, CO["triu"][0]:CO["triu"][0] + 128] = (p <= f)
    c[:, CO["negmask"][0]:CO["negmask"][0] + 128] = np.where(p <= f, 0.0, -30000.0)
    o = CO["dilmask"][0]
    c[:, o:o + 128] = np.where(p <= f, 0.0, -30000.0)
    c[:, o + 128:o + 256] = np.where(p >= f, 0.0, -30000.0)
    inv_freq = (500000.0 ** (-np.arange(0, 16, 2, dtype=np.float32) / np.float32(16))).astype(np.float32)
    c[:, CO["invf"][0]:CO["invf"][0] + 8] = inv_freq[None, :]
    return c


class Buf:
    __slots__ = ("name", "h", "last_w", "readers", "excl")

    def __init__(self, name, h, excl=False):
        self.name = name
        self.h = h
        self.excl = excl
        self.last_w = None
        self.readers = {}

    def __getitem__(self, idx):
        return self.h[idx]


class Sched:
    def __init__(self, nc, stack):
        self.nc = nc
        self.stack = stack
        self.eng = {"pe": nc.tensor, "dve": nc.vector, "act": nc.scalar, "pool": nc.gpsimd, "sp": nc.sync}
        self.sem = {}
        self.cnt = {}
        self.seen = {}
        for e in self.eng:
            self.sem[e] = stack.enter_context(nc.semaphore("s_" + e))
            self.cnt[e] = 0
            self.seen[e] = {}
        self.nwaits = 0
        self.dma_i = {}
        self.yield_hook = None
        self.ninstr = 0

    def sbuf(self, name, shape, dtype):
        h = self.stack.enter_context(self.nc.sbuf_tensor(name, list(shape), dtype))
        return Buf(name, h)

    def psum(self, name, shape, dtype):
        h = self.stack.enter_context(self.nc.psum_tensor(name, list(shape), dtype))
        return Buf(name, h, excl=True)

    def dram(self, name, shape, dtype, kind="Internal"):
        h = self.nc.dram_tensor(name, list(shape), dtype, kind=kind)
        return Buf(name, h.ap())

    def dma_stream(self, name):
        return "dma_" + name

    def _wait(self, eng, deps):
        e = self.eng[eng]
        seen = self.seen[eng]
        for s, v in deps.items():
            if v <= 0:
                continue
            if eng == "pe" and s == "pe":
                continue
            if s.startswith("dma_"):
                v = self.cnt[s]
            if seen.get(s, 0) >= v:
                continue
            e.wait_ge(self.sem[s], v)
            seen[s] = v
            self.nwaits += 1

    def _deps(self, reads, writes):
        deps = {}

        def add(sv):
            if sv is None:
                return
            s, v = sv
            if deps.get(s, 0) < v:
                deps[s] = v
        for b in reads:
            add(b.last_w)
            if b.excl:
                for s, v in b.readers.items():
                    add((s, v))
        for b in writes:
            add(b.last_w)
            for s, v in b.readers.items():
                add((s, v))
        return deps

    def op(self, eng, fn, reads=(), writes=(), inc=True):
        deps = self._deps(reads, writes)
        self._wait(eng, deps)
        ins = fn()
        if inc:
            self.cnt[eng] += 1
            ins.then_inc(self.sem[eng], 1)
            c = self.cnt[eng]
        else:
            assert eng == "pe"
            c = self.cnt[eng] + 1
        for b in reads:
            b.readers[eng] = c
        for b in writes:
            b.last_w = (eng, c)
            b.readers = {}
        self.ninstr += 1
        if self.yield_hook is not None:
            self.yield_hook()
        return ins

    def dma(self, q, stream, out_ap, in_ap, reads=(), writes=(), rot=4, chain=True, **kw):
        n = self.dma_i.get(stream, 0)
        self.dma_i[stream] = n + 1
        sub = f"{stream}#{n % rot}"
        if sub not in self.sem:
            self.sem[sub] = self.stack.enter_context(self.nc.semaphore("s_" + sub.replace("#", "_")))
            self.cnt[sub] = 0
        deps = self._deps(reads, writes)
        if chain and self.cnt[sub] > 0:
            deps[sub] = self.cnt[sub]
        self._wait(q, deps)
        ins = self.eng[q].dma_start(out=out_ap, in_=in_ap, **kw)
        self.cnt[sub] += 16
        ins.then_inc(self.sem[sub], 16)
        c = self.cnt[sub]
        for b in reads:
            b.readers[sub] = c
        for b in writes:
            b.last_w = (sub, c)
            b.readers = {}
        self.ninstr += 1
        return ins

    def finish(self, out_bufs, eng="sp"):
        deps = {s: v for s, v in self.cnt.items() if v > 0}
        self._wait(eng, deps)


class Rot:
    def __init__(self, bufs):
        self.bufs = bufs
        self.i = 0

    def next(self):
        b = self.bufs[self.i % len(self.bufs)]
        self.i += 1
        return b


class Cfg:
    def __init__(self, T=2048, NSEQ=2, NL=4, n_exp=129, mixers=("gla", "sgu", "ssd", "dil"), dbg=False):
        self.T = T
        self.NT = T // 128
        self.NG = T // 512
        self.NSEQ = NSEQ
        self.NL = NL
        self.n_exp = n_exp
        self.mixers = mixers
        self.dbg = dbg


class MK:
    def __init__(self, cfg):
        self.cfg = cfg

    def bank(self):
        b = self.PS[self.ps_i % 8]
        self.ps_i += 1
        return b

    def tmp(self, name, shape, dtype, n=2, stack=None):
        st = stack if stack is not None else self.st
        bufs = []
        for i in range(n):
            nm = self.uniq(f"{name}{i}")
            h = st.enter_context(self.nc.sbuf_tensor(nm, list(shape), dtype))
            bufs.append(Buf(nm, h))
        return Rot(bufs)

    def uniq(self, name):
        self._uid = getattr(self, "_uid", 0) + 1
        return f"{name}_u{self._uid}"

    def alloc(self, name, shape, dtype, stack=None):
        st = stack if stack is not None else self.st
        name = self.uniq(name)
        h = st.enter_context(self.nc.sbuf_tensor(name, list(shape), dtype))
        return Buf(name, h)

    def zip_run(self, fns):
        import threading
        n = len(fns)
        if n == 1:
            fns[0]()
            return
        batons = [threading.Semaphore(0) for _ in range(n)]
        alive = [True] * n
        errs = []
        tl = threading.local()

        def nxt(i):
            for d in range(1, n + 1):
                j = (i + d) % n
                if alive[j]:
                    return j
            return None

        def hook():
            i = getattr(tl, "idx", None)
            if i is None:
                return
            j = nxt(i)
            if j is not None and j != i:
                batons[j].release()
                batons[i].acquire()

        def runner(i):
            tl.idx = i
            batons[i].acquire()
            try:
                fns[i]()
            except BaseException as e:
                errs.append(e)
            finally:
                alive[i] = False
                j = nxt(i)
                if j is not None:
                    batons[j].release()
                else:
                    done.release()
        done = threading.Semaphore(0)
        old = self.S.yield_hook
        self.S.yield_hook = hook
        ths = [threading.Thread(target=runner, args=(i,)) for i in range(n)]
        for t in ths:
            t.start()
        batons[0].release()
        done.acquire()
        for t in ths:
            t.join()
        self.S.yield_hook = old
        if errs:
            raise errs[0]

    def barrier(self):
        S = self.S
        deps = {s: v for s, v in S.cnt.items() if v > 0}
        for e in ("pe", "dve", "act", "pool", "sp"):
            S._wait(e, dict(deps))

    def mm(self, out, lhsT, rhs, start, stop, reads, writes, inc=True):
        nc = self.nc
        return self.S.op("pe", lambda: nc.tensor.matmul(out, lhsT=lhsT, rhs=rhs, start=start, stop=stop),
                         reads=reads, writes=writes, inc=inc)

    def tr(self, out, in_, reads, writes):
        nc = self.nc
        idn = self.ident
        n = in_.shape[0]
        return self.S.op("pe", lambda: nc.tensor.transpose(out=out, in_=in_, identity=idn[0:n, 0:n]),
                         reads=list(reads) + [self.CONST], writes=writes)

    def act(self, out, in_, func, reads, writes, **kw):
        nc = self.nc
        return self.S.op("act", lambda: nc.scalar.activation(out=out, in_=in_, func=func, **kw),
                         reads=reads, writes=writes)

    def tt(self, out, in0, in1, op, reads, writes, eng="dve"):
        e = self.S.eng[eng]
        return self.S.op(eng, lambda: e.tensor_tensor(out=out, in0=in0, in1=in1, op=op), reads=reads, writes=writes)

    def ts(self, out, in0, s1, op0, reads, writes, s2=None, op1=None, eng="dve", **kw):
        e = self.S.eng[eng]
        if op1 is None:
            return self.S.op(eng, lambda: e.tensor_scalar(out=out, in0=in0, scalar1=s1, scalar2=None, op0=op0, **kw),
                             reads=reads, writes=writes)
        return self.S.op(eng, lambda: e.tensor_scalar(out=out, in0=in0, scalar1=s1, scalar2=s2, op0=op0, op1=op1, **kw),
                         reads=reads, writes=writes)

    def stt(self, out, in0, scalar, in1, op0, op1, reads, writes, eng="dve"):
        e = self.S.eng[eng]
        return self.S.op(eng, lambda: e.scalar_tensor_tensor(out=out, in0=in0, scalar=scalar, in1=in1, op0=op0, op1=op1),
                         reads=reads, writes=writes)

    def cp(self, out, in_, reads, writes, eng="dve"):
        if eng == "act":
            return self.act(out, in_, AF.Copy, reads, writes)
        e = self.S.eng[eng]
        return self.S.op(eng, lambda: e.tensor_copy(out=out, in_=in_), reads=reads, writes=writes)

    def memset(self, ap, val, writes, eng="dve"):
        e = self.S.eng[eng]
        return self.S.op(eng, lambda: e.memset(ap, val), writes=writes)

    def load(self, out_ap, in_ap, writes, stream="ld", q="sp", reads=()):
        return self.S.dma(q, self.S.dma_stream(stream), out_ap, in_ap, reads=reads, writes=writes)

    def loadw(self, out_ap, in_ap, writes, stream, rot=2, chain=True):
        return self.S.dma("pool", self.S.dma_stream(stream), out_ap, in_ap, writes=writes, rot=rot, chain=chain)

    def build(self):
        cfg = self.cfg
        nc = bass.Bass("TRN2", target_bir_lowering=False)
        self.nc = nc
        T, NT, NSEQ, NL = cfg.T, cfg.NT, cfg.NSEQ, cfg.NL

        def din(name, shape, dt=F32):
            return nc.dram_tensor(name, list(shape), dt, kind="ExternalInput").ap()
        self.d_x = din("x", [NSEQ * T, D])
        self.d_pos = din("pos", [NSEQ * 128, NT], I32)
        self.d_win = din("w_in", [NL * D, NIN])
        self.d_wout = din("w_out", [NL * D, D])
        self.d_rw = din("router_w", [NL * D, NE])
        import os
        tiny = os.environ.get("STOP", "") in ("mix", "load")
        self.d_wg = din("exp_w_gate", [8 if tiny else NL * NE * D, 256])
        self.d_wu = din("exp_w_up", [8 if tiny else NL * NE * D, 256])
        self.d_wd = din("exp_w_down", [8 if tiny else NL * NE * 256, D])
        self.d_swg = din("sh_w_gate", [NL * D, 256])
        self.d_swu = din("sh_w_up", [NL * D, 256])
        self.d_swd = din("sh_w_down", [NL * 256, D])
        self.d_wgate = din("gla_w_gate", [NL * 16, 128])
        self.d_sguwT = din("sgu_wT", [NL * 4 * 128, 128])
        self.d_pvec = din("pvec", [NL * 128, NPV])
        self.d_pcol = din("pcol", [NL * 128, NPC])
        self.d_const = din("consts", [128, NCONST])
        self.d_out = nc.dram_tensor("out", [NSEQ * T, D], F32, kind="ExternalOutput").ap()
        self.dbg = {}
        if cfg.dbg:
            for nm, shp in [("dbg_yT", [D, T]), ("dbg_h", [T, D]), ("dbg_G", [T, NE])]:
                self.dbg[nm] = nc.dram_tensor(nm, shp, F32, kind="ExternalOutput").ap()

        with ExitStack() as st:
            self.st = st
            S = Sched(nc, st)
            self.S = S
            self.PS = [S.psum(f"ps{i}", [128, 512], F32) for i in range(8)]
            self.ps_i = 0
            self.Xh = st.enter_context(nc.sbuf_tensor("X", [128, NT, D], F32))
            self.X = [Buf(f"X{t}", self.Xh[:, t, :]) for t in range(NT)]
            self.XTh = st.enter_context(nc.sbuf_tensor("XT", [128, KD, T], BF16))
            self.XT = [Buf(f"XT{g}", self.XTh[:, :, g * 512:(g + 1) * 512]) for g in range(cfg.NG)]
            self.CONST = self.alloc("CONST", [128, NCONST], F32)
            self.PSM = self.alloc("PSM", [128, NPSM], F32)
            self.PCOL = self.alloc("PCOL", [128, NPC], F32)
            self.Gh = st.enter_context(nc.sbuf_tensor("G", [128, NT, NE + 1], F32))
            self.G = Buf("G", self.Gh)
            self.memset(self.Gh[:, :, :], 1.0, writes=[self.G])
            self.setup_consts()
            import os
            self.stop = os.environ.get("STOP", "")
            for s in range(NSEQ):
                self.load_seq(s)
                if self.stop == "load":
                    break
                for l in range(NL):
                    self.layer(s, l)
            S.finish([])
            print("instrs", S.ninstr, "waits", S.nwaits, {k: v for k, v in S.cnt.items()})
        return nc

    def cst(self, name):
        o, w = CO[name]
        return self.CONST[:, o:o + w]

    def psm(self, name):
        o, w = PV[name]
        return self.PSM[:, o - PSM0:o - PSM0 + w]

    def setup_consts(self):
        self.load(self.CONST[:], self.d_const[:, :], writes=[self.CONST])
        self.ident = self.cst("ident")
        self.IDB = self.alloc("IDB", [128, 128], BF16)
        self.cp(self.IDB[:], self.ident, reads=[self.CONST], writes=[self.IDB])
        self.DMASK = self.alloc("DMASK", [128, 256], BF16)
        self.cp(self.DMASK[:], self.cst("dilmask"), reads=[self.CONST], writes=[self.DMASK])

    def load_seq(self, s):
        cfg = self.cfg
        for t in range(cfg.NT):
            r0 = s * cfg.T + t * 128
            self.load(self.X[t][:], self.d_x[r0:r0 + 128, :], writes=[self.X[t]], stream="xin")
            self.transpose_tile(t)
        if "dil" in cfg.mixers:
            self.rope_tables(s)

    def transpose_tile(self, t, htf=None):
        g = t // 4
        c0 = (t % 4) * 128
        for half in range(2):
            ps = self.bank()
            for j in range(4):
                k = half * 4 + j
                self.tr(ps[:, j * 128:(j + 1) * 128], self.X[t][:, k * 128:(k + 1) * 128], reads=[self.X[t]], writes=[ps])
            src = ps[:, :].rearrange("p (a b) -> p a b", a=4)
            dst = self.XT[g][:, half * 4:half * 4 + 4, c0:c0 + 128]
            if half == 0:
                self.cp(dst, src, reads=[ps], writes=[self.XT[g]], eng="act")
            else:
                self.cp(dst, src, reads=[ps], writes=[self.XT[g]], eng="dve")
            if htf is not None:
                self.cp(htf[:, half * 4:half * 4 + 4, :], src, reads=[ps], writes=[htf], eng=("dve" if half == 0 else "act"))

    def proj_tok(self, t, W, c0, n, ps_ap, ps):
        g = t // 4
        tc0 = (t % 4) * 128
        for k in range(KD):
            self.mm(ps_ap, self.XT[g][:, k, tc0:tc0 + 128], W[:, k, c0:c0 + n], k == 0, k == KD - 1,
                    reads=[self.XT[g], W], writes=[ps], inc=(k == KD - 1))

    def proj_feat(self, g, W, c0, m, ps_ap, ps):
        for k in range(KD):
            self.mm(ps_ap, W[:, k, c0:c0 + m], self.XT[g][:, k, :], k == 0, k == KD - 1,
                    reads=[self.XT[g], W], writes=[ps], inc=(k == KD - 1))

    def win_ap(self, l, c0, c1):
        return self.d_win[l * D:(l + 1) * D, :].rearrange("(k p) n -> p k n", p=128)[:, :, c0:c1]

    def layer(self, s, l):
        cfg = self.cfg
        nc = self.nc
        self.load(self.PSM[:], self.d_pvec[l * 128:(l + 1) * 128, PSM0:NPV], writes=[self.PSM])
        self.load(self.PCOL[:], self.d_pcol[l * 128:(l + 1) * 128, :], writes=[self.PCOL])
        with ExitStack() as mst:
            YTh = mst.enter_context(nc.sbuf_tensor(self.uniq("YT"), [128, KD, cfg.T], BF16))
            self.YTh = YTh
            self.YT = [Buf(f"YT{m}", YTh[:, 2 * m:2 * m + 2, :]) for m in range(4)]
            names = ["gla", "sgu", "ssd", "dil"]
            for m, nm in enumerate(names):
                if nm not in cfg.mixers:
                    self.memset(self.YT[m][:], 0.0, writes=[self.YT[m]], eng="pool")
            for grp in (("gla", "sgu"), ("ssd",), ("dil",)):
                grp = [nm for nm in grp if nm in cfg.mixers]
                if not grp:
                    continue
                with ExitStack() as sst:
                    self.zip_run([lambda nm=nm: getattr(self, "mix_" + nm)(s, l, sst, self.YT[names.index(nm)]) for nm in grp])
                    self.barrier()
            if cfg.dbg and s == 0 and l == 0:
                self.dump_yT()
            if self.stop == "mix":
                return
            with ExitStack() as sst:
                self.outproj_ln_router(s, l, sst)
                self.barrier()
        if self.stop == "outproj":
            return
        with ExitStack() as sst:
            self.moe(s, l, sst)
            self.barrier()

    def dump_yT(self):
        cfg = self.cfg
        with ExitStack() as sst:
            tmp = self.alloc("dbgy", [128, cfg.T], F32, stack=sst)
            for k in range(KD):
                self.cp(tmp[:], self.YTh[:, k, :], reads=[self.YT[k // 2]], writes=[tmp])
                self.load(self.dbg["dbg_yT"][k * 128:(k + 1) * 128, :], tmp[:], writes=[], reads=[tmp], stream="dbg")
            self.barrier()


    def layernorm(self, src, dst, g_ap, b_ap, n, eps, reads, preads, writes, st, tmpn):
        nc = self.nc
        self.S.op("dve", lambda: nc.vector.reduce_sum(out=st[:, 0:1], in_=src, axis=AX.X), reads=reads, writes=[st])
        self.ts(st[:, 1:2], st[:, 0:1], -1.0 / n, ALU.mult, reads=[st], writes=[st])
        self.act(tmpn[:, 0:n], src, AF.Square, reads=list(reads) + [st], writes=[tmpn, st], bias=st[:, 1:2], scale=1.0,
                 accum_out=st[:, 2:3])
        self.act(st[:, 3:4], st[:, 2:3], AF.Sqrt, reads=[st], writes=[st], scale=1.0 / n, bias=float(eps))
        self.S.op("dve", lambda: nc.vector.reciprocal(out=st[:, 4:5], in_=st[:, 3:4]), reads=[st], writes=[st])
        self.ts(tmpn[:, 0:n], src, st[:, 1:2], ALU.add, reads=list(reads) + [st], writes=[tmpn], s2=st[:, 4:5], op1=ALU.mult)
        self.tt(tmpn[:, 0:n], tmpn[:, 0:n], g_ap, ALU.mult, reads=[tmpn] + list(preads), writes=[tmpn])
        self.tt(dst, tmpn[:, 0:n], b_ap, ALU.add, reads=[tmpn] + list(preads), writes=writes)

    def mix_gla(self, s, l, st, YTm):
        cfg = self.cfg
        nc = self.nc
        W = self.alloc("Wgla", [128, KD, 784], BF16, stack=st)
        self.loadw(W[:], self.win_ap(l, 0, 784), writes=[W], stream="win")
        WGf = self.alloc("WGf", [16, 128], F32, stack=st)
        self.load(WGf[:], self.d_wgate[l * 16:(l + 1) * 16, :], writes=[WGf])
        NBG = self.alloc("NBG", [128, 1], F32, stack=st)
        self.ts(NBG[:], self.PCOL[:, 0:1], -1.0, ALU.mult, reads=[self.PCOL], writes=[NBG])
        Qbd = self.alloc("Qbd", [128, 4, 512], BF16, stack=st)
        self.memset(Qbd[:], 0.0, writes=[Qbd], eng="pool")
        QT = self.alloc("glQT", [128, 512], F32, stack=st)
        KTf = self.alloc("glKTf", [128, 512], F32, stack=st)
        LR = self.alloc("glLR", [16, 512], F32, stack=st)
        SP = self.alloc("glSP", [128, 512], F32, stack=st)
        CS = self.alloc("glCS", [128, 512], F32, stack=st)
        EQ = self.alloc("glEQ", [128, 512], F32, stack=st)
        EK = self.alloc("glEK", [128, 512], F32, stack=st)
        KT = self.alloc("glKT", [128, 512], BF16, stack=st)
        NCL = self.alloc("glNCL", [128, 4], F32, stack=st)
        EKI = self.tmp("glEKI", [128, 128], F32, stack=st)
        KINr = self.tmp("glKIN", [128, 128], BF16, stack=st)
        S32 = self.alloc("glS32", [128, 64], F32, stack=st)
        Sb = self.alloc("glSb", [128, 64], BF16, stack=st)
        self.memset(S32[:], 0.0, writes=[S32])
        self.memset(Sb[:], 0.0, writes=[Sb])
        Vr = self.tmp("glV", [128, 256], BF16, stack=st)
        SRr = self.tmp("glSR", [128, 256], F32, n=1, stack=st)
        ATr = self.tmp("glAT", [128, 512], BF16, n=1, stack=st)
        OSr = self.tmp("glOS", [128, 256], F32, n=1, stack=st)
        SQr = self.tmp("glSQ", [128, 256], F32, n=1, stack=st)
        STr = self.tmp("glST", [128, 8], F32, stack=st)
        Yr = self.tmp("glY", [128, 256], F32, n=1, stack=st)
        triu4 = self.cst("triu").unsqueeze(1).to_broadcast([128, 4, 128])
        for g in range(cfg.NG):
            psq, psk, psl = self.bank(), self.bank(), self.bank()
            self.proj_feat(g, W, O_AQ, 128, psq[:, :], psq)
            self.proj_feat(g, W, O_AK, 128, psk[:, :], psk)
            self.proj_feat(g, W, O_ALR, 16, psl[0:16, :], psl)
            self.cp(QT[:], psq[:, :], reads=[psq], writes=[QT], eng="act")
            self.cp(KTf[:], psk[:, :], reads=[psk], writes=[KTf], eng="dve")
            self.cp(LR[:], psl[0:16, :], reads=[psl], writes=[LR], eng="act")
            psz = self.bank()
            self.mm(psz[:, :], WGf[:], LR[:], True, True, reads=[WGf, LR], writes=[psz])
            self.act(SP[:], psz[:, :], AF.Exp, reads=[psz, NBG], writes=[SP], scale=-1.0, bias=NBG[:, 0:1])
            self.act(SP[:], SP[:], AF.Ln, reads=[SP], writes=[SP], bias=1.0)
            for c in range(4):
                sl = slice(c * 128, (c + 1) * 128)
                self.S.op("dve", lambda: nc.vector.tensor_tensor_scan(out=CS[:, sl], data0=SP[:, sl], data1=SP[:, sl], initial=0.0,
                                                                     op0=ALU.add, op1=ALU.bypass), reads=[SP], writes=[CS])
            self.act(EQ[:], CS[:], AF.Exp, reads=[CS], writes=[EQ], scale=-1.0 / 16)
            self.act(EK[:], CS[:], AF.Exp, reads=[CS], writes=[EK], scale=1.0 / 16)
            for h in range(4):
                ps_ = slice(32 * h, 32 * h + 32)
                self.stt(Qbd[ps_, h, :], QT[ps_, :], 32.0 ** -0.5, EQ[ps_, :], ALU.mult, ALU.mult, reads=[QT, EQ], writes=[Qbd])
            self.tt(KT[:], KTf[:], EK[:], ALU.mult, reads=[KTf, EK], writes=[KT])
            self.ts(NCL[:].unsqueeze(2), CS[:, :].rearrange("p (c i) -> p c i", c=4)[:, :, 127:128], -1.0 / 16, ALU.mult,
                    reads=[CS], writes=[NCL])
            for c in range(4):
                t = 4 * g + c
                sl = slice(c * 128, (c + 1) * 128)
                psvr = self.bank()
                self.proj_tok(t, W, O_AV, 512, psvr[:, :], psvr)
                V, SR = Vr.next(), SRr.next()
                self.cp(V[:], psvr[:, 0:256], reads=[psvr], writes=[V], eng="dve")
                self.act(SR[:], psvr[:, 256:512], AF.Silu, reads=[psvr], writes=[SR])
                eki, KIN = EKI.next(), KINr.next()
                self.act(eki[:], CS[:, sl], AF.Exp, reads=[CS, NCL], writes=[eki], scale=1.0 / 16, bias=NCL[:, c:c + 1])
                self.tt(eki[:], eki[:], KTf[:, sl], ALU.mult, reads=[eki, KTf], writes=[eki])
                pst = self.bank()
                self.tr(pst[:, 0:128], eki[:], reads=[eki], writes=[pst])
                self.cp(KIN[:], pst[:, 0:128], reads=[pst], writes=[KIN], eng="act")
                psa = self.bank()
                self.mm(psa[:, :].rearrange("p (h i) -> p h i", h=4), KT[:, sl], Qbd[:, :, sl], True, True, reads=[KT, Qbd], writes=[psa])
                AT = ATr.next()
                self.tt(AT[:, :].rearrange("p (h i) -> p h i", h=4), psa[:, :].rearrange("p (h i) -> p h i", h=4), triu4, ALU.mult,
                        reads=[psa, self.CONST], writes=[AT])
                pso = self.bank()
                for h in range(4):
                    self.mm(pso[:, h * 64:(h + 1) * 64], AT[:, h * 128:(h + 1) * 128], V[:, h * 64:(h + 1) * 64], True, False,
                            reads=[AT, V], writes=[pso])
                    self.mm(pso[:, h * 64:(h + 1) * 64], Qbd[:, h, sl], Sb[:], False, True, reads=[Qbd, Sb], writes=[pso])
                pss = self.bank()
                self.mm(pss[:, 0:256], KIN[:], V[:], True, True, reads=[KIN, V], writes=[pss])
                for h in range(4):
                    ps_ = slice(32 * h, 32 * h + 32)
                    self.stt(S32[ps_, :], S32[ps_, :], EQ[ps_, c * 128 + 127:c * 128 + 128], pss[ps_, h * 64:(h + 1) * 64],
                             ALU.mult, ALU.add, reads=[S32, EQ, pss], writes=[S32])
                self.cp(Sb[:], S32[:], reads=[S32], writes=[Sb], eng="pool")
                OS, SQ, ST, Y = OSr.next(), SQr.next(), STr.next(), Yr.next()
                self.cp(OS[:], pso[:, 0:256], reads=[pso], writes=[OS], eng="act")
                self.tt(SQ[:], OS[:], OS[:], ALU.mult, reads=[OS], writes=[SQ], eng="pool")
                self.S.op("dve", lambda: nc.vector.tensor_reduce(out=ST[:, 0:4], in_=SQ[:, :].rearrange("p (h v) -> p h v", h=4),
                                                                 axis=AX.X, op=ALU.add), reads=[SQ], writes=[ST])
                self.act(ST[:, 0:4], ST[:, 0:4], AF.Sqrt, reads=[ST], writes=[ST], scale=1.0 / 64, bias=float(RMS_EPS))
                self.S.op("dve", lambda: nc.vector.reciprocal(out=ST[:, 4:8], in_=ST[:, 0:4]), reads=[ST], writes=[ST])
                self.tt(Y[:, :].rearrange("p (h v) -> p h v", h=4), OS[:, :].rearrange("p (h v) -> p h v", h=4),
                        ST[:, 4:8].unsqueeze(2).to_broadcast([128, 4, 64]), ALU.mult, reads=[OS, ST], writes=[Y])
                self.tt(Y[:], Y[:], self.psm("gla_nw"), ALU.mult, reads=[Y, self.PSM], writes=[Y])
                self.tt(Y[:], Y[:], SR[:], ALU.mult, reads=[Y, SR], writes=[Y])
                self.emit_yT(Y, YTm, t)

    def mix_sgu(self, s, l, st, YTm):
        cfg = self.cfg
        nc = self.nc
        W = self.alloc("Wsgu", [128, KD, 512], BF16, stack=st)
        self.loadw(W[:], self.win_ap(l, O_BU, O_BU + 512), writes=[W], stream="win")
        WSf = self.alloc("WSf", [128, 4, 128], F32, stack=st)
        WS = self.alloc("WS", [128, 4, 128], BF16, stack=st)
        self.load(WSf[:], self.d_sguwT[l * 512:(l + 1) * 512, :].rearrange("(g j) i -> j g i", j=128), writes=[WSf])
        self.tt(WS[:], WSf[:], self.cst("triu").unsqueeze(1).to_broadcast([128, 4, 128]), ALU.mult,
                reads=[WSf, self.CONST], writes=[WS])
        Ub = self.tmp("sgU", [128, 256], F32, stack=st)
        Vg = self.tmp("sgV", [128, 256], F32, stack=st)
        Vt = self.tmp("sgT", [128, 256], F32, stack=st)
        VNb = self.tmp("sgN", [128, 256], BF16, stack=st)
        STt = self.tmp("sgS", [128, 8], F32, stack=st)
        YB = self.tmp("sgY", [128, 256], F32, stack=st)
        def tile(t, par):
            ps = self.bank()
            self.proj_tok(t, W, 0, 512, ps[:, 0:512], ps)
            U, V, VT, VN, ST, Y = [r.bufs[par] for r in (Ub, Vg, Vt, VNb, STt, YB)]
            self.act(U[:], ps[:, 0:256], AF.Gelu, reads=[ps], writes=[U])
            self.act(V[:], ps[:, 256:512], AF.Gelu, reads=[ps], writes=[V])
            self.layernorm(V[:], VN[:], self.psm("sgu_g"), self.psm("sgu_b"), 256, LN_EPS, reads=[V], preads=[self.PSM],
                           writes=[VN], st=ST, tmpn=VT)
            ps2 = self.bank()
            for g in range(4):
                self.mm(ps2[:, g * 64:(g + 1) * 64], WS[:, g, :], VN[:, g * 64:(g + 1) * 64], True, True,
                        reads=[WS, VN], writes=[ps2])
            self.tt(Y[:], ps2[:, 0:256], self.psm("sgu_bs"), ALU.add, reads=[ps2, self.PSM], writes=[Y])
            self.tt(Y[:], Y[:], U[:], ALU.mult, reads=[Y, U], writes=[Y])
            self.emit_yT(Y, YTm, t)
        if self.S.yield_hook is None:
            self.zip_run([lambda p=p: [tile(t, p) for t in range(p, cfg.NT, 2)] for p in range(2)])
        else:
            for t in range(cfg.NT):
                tile(t, t % 2)

    def emit_yT(self, Y, YTm, t):
        ps3 = self.bank()
        for c in range(2):
            self.tr(ps3[:, c * 128:(c + 1) * 128], Y[:, c * 128:(c + 1) * 128], reads=[Y], writes=[ps3])
        self.cp(YTm[:, :, t * 128:(t + 1) * 128], ps3[:, 0:256].rearrange("p (a b) -> p a b", a=2), reads=[ps3],
                writes=[YTm], eng="act")

    def mix_ssd(self, s, l, st, YTm):
        cfg = self.cfg
        nc = self.nc
        W = self.alloc("Wssd", [128, KD, 772], BF16, stack=st)
        self.loadw(W[:], self.win_ap(l, O_CZ, O_CZ + 772), writes=[W], stream="win")
        AN = self.alloc("sdAN", [128, 4], F32, stack=st)
        self.act(AN[:], self.psm("a_log"), AF.Exp, reads=[self.PSM], writes=[AN])
        self.ts(AN[:], AN[:], -1.0, ALU.mult, reads=[AN], writes=[AN])
        XBC = self.alloc("sdXBC", [128, 4, 515], F32, stack=st)
        self.memset(XBC[:], 0.0, writes=[XBC], eng="pool")
        CAr = self.tmp("sdCA", [128, 512], F32, stack=st)
        XST = self.alloc("sdXST", [128, 2, 512], F32, stack=st)
        BTf = self.alloc("sdBTf", [128, 512], F32, stack=st)
        BTb = self.alloc("sdBTb", [128, 2, 512], BF16, stack=st)
        self.memset(BTb[:], 0.0, writes=[BTb], eng="pool")
        CTf = self.alloc("sdCTf", [128, 512], F32, stack=st)
        CTb = self.alloc("sdCTb", [128, 512], BF16, stack=st)
        S32 = self.alloc("sdS32", [128, 128], F32, stack=st)
        Sb = self.alloc("sdSb", [128, 128], BF16, stack=st)
        self.memset(S32[:], 0.0, writes=[S32])
        self.memset(Sb[:], 0.0, writes=[Sb])
        SZr = self.tmp("sdSZ", [128, 256], F32, stack=st)
        DTr = self.tmp("sdDT", [128, 24], F32, stack=st)
        for _b in DTr.bufs:
            self.memset(_b[:], 0.0, writes=[_b])
        XSr = self.tmp("sdXS", [128, 256], F32, stack=st)
        BKr = self.tmp("sdBK", [128, 128], BF16, stack=st)
        XDr = self.tmp("sdXD", [128, 256], BF16, stack=st)
        XDSr = self.tmp("sdXDS", [128, 256], BF16, stack=st)
        DABr = self.tmp("sdDAB", [128, 512], F32, n=1, stack=st)
        EARr = self.tmp("sdEAR", [128, 512], F32, stack=st)
        DFr = self.tmp("sdDF", [128, 512], F32, n=1, stack=st)
        WTr = self.tmp("sdWT", [128, 512], BF16, stack=st)
        CSTr = self.tmp("sdCST", [128, 512], BF16, stack=st)
        for _b in CSTr.bufs:
            self.memset(_b[:], 0.0, writes=[_b], eng="pool")
        Y1r = self.tmp("sdY1", [128, 256], F32, n=1, stack=st)
        SQr = self.tmp("sdSQ", [128, 256], F32, n=1, stack=st)
        STr = self.tmp("sdST", [128, 8], F32, stack=st)
        Yr = self.tmp("sdY", [128, 256], F32, stack=st)
        triu = self.cst("triu")
        negmask = self.cst("negmask")
        for g in range(cfg.NG):
            if g > 0:
                self.cp(XBC[:, :, 0:3], XBC[:, :, 512:515], reads=[XBC], writes=[XBC], eng="pool")
            for ch in range(4):
                ps = self.bank()
                self.proj_feat(g, W, 256 + ch * 128, 128, ps[:, :], ps)
                self.cp(XBC[:, ch, 3:515], ps[:, :], reads=[ps], writes=[XBC], eng=("act" if ch % 2 == 0 else "dve"))
            for ch in range(4):
                CA = CAr.next()
                eng = "dve"
                wc = lambda w: self.PCOL[:, 1 + ch * 4 + w:2 + ch * 4 + w]
                self.ts(CA[:], XBC[:, ch, 0:512], wc(0), ALU.mult, reads=[XBC, self.PCOL], writes=[CA], eng=eng)
                for w in range(1, 4):
                    self.stt(CA[:], XBC[:, ch, w:w + 512], wc(w), CA[:], ALU.mult, ALU.add, reads=[XBC, self.PCOL, CA], writes=[CA], eng=eng)
                dst, dbuf = [(XST[:, 0, :], XST), (XST[:, 1, :], XST), (BTf[:], BTf), (CTf[:], CTf)][ch]
                self.act(dst, CA[:], AF.Silu, reads=[CA, self.PCOL], writes=[dbuf], bias=self.PCOL[:, 17 + ch:18 + ch], scale=1.0)
            self.cp(BTb[0:64, 0, :], BTf[0:64, :], reads=[BTf], writes=[BTb], eng="pool")
            self.cp(BTb[64:128, 1, :], BTf[64:128, :], reads=[BTf], writes=[BTb], eng="pool")
            self.cp(CTb[:], CTf[:], reads=[CTf], writes=[CTb], eng="pool")
            import os
            L3 = float(os.environ.get("STOP3", "99"))
            for c in range(4):
                if L3 < 1:
                    continue
                t = 4 * g + c
                sl = slice(c * 128, (c + 1) * 128)
                psz = self.bank()
                self.proj_tok(t, W, 0, 256, psz[:, 0:256], psz)
                self.proj_tok(t, W, 768, 4, psz[:, 256:260], psz)
                SZ, DT = SZr.next(), DTr.next()
                self.act(SZ[:], psz[:, 0:256], AF.Silu, reads=[psz], writes=[SZ])
                self.tt(DT[:, 0:4], psz[:, 256:260], self.psm("dt_bias"), ALU.add, reads=[psz, self.PSM], writes=[DT])
                self.act(DT[:, 0:4], DT[:, 0:4], AF.Exp, reads=[DT], writes=[DT])
                self.act(DT[:, 4:8], DT[:, 0:4], AF.Ln, reads=[DT], writes=[DT], bias=1.0)
                self.tt(DT[:, 8:12], DT[:, 4:8], AN[:], ALU.mult, reads=[DT, AN], writes=[DT])
                if L3 < 2:
                    continue
                pst = self.bank()
                for j in range(2):
                    self.tr(pst[:, j * 128:(j + 1) * 128], XST[:, j, sl], reads=[XST], writes=[pst])
                self.tr(pst[:, 256:384], BTf[:, sl], reads=[BTf], writes=[pst])
                XS, BK, XD, XDS = XSr.next(), BKr.next(), XDr.next(), XDSr.next()
                self.cp(XS[:], pst[:, 0:256], reads=[pst], writes=[XS], eng="act")
                self.cp(BK[:], pst[:, 256:384], reads=[pst], writes=[BK], eng="dve")
                xs4 = XS[:, :].rearrange("p (h v) -> p h v", h=4)
                self.tt(XD[:, :].rearrange("p (h v) -> p h v", h=4), xs4, DT[:, 4:8].unsqueeze(2).to_broadcast([128, 4, 64]),
                        ALU.mult, reads=[XS, DT], writes=[XD])
                if L3 < 3:
                    continue
                DAB = DABr.next()
                self.cp(DAB[:, :].rearrange("p (h i) -> p h i", h=4), DT[:, 8:12].unsqueeze(2).to_broadcast([128, 4, 128]),
                        reads=[DT], writes=[DAB], eng="dve")
                psr, psc = self.bank(), self.bank()
                for h in range(4):
                    self.mm(psr[:, h * 128:(h + 1) * 128], DAB[:, h * 128:(h + 1) * 128], triu, True, True,
                            reads=[DAB, self.CONST], writes=[psr])
                if L3 < 3.2:
                    continue
                self.mm(psc[:, 0:16], triu, DT[:, 8:24], True, True, reads=[DT, self.CONST], writes=[psc])
                self.cp(DT[:, 12:16], psc[:, 0:4], reads=[psc], writes=[DT], eng="dve")
                if L3 < 3.4:
                    continue
                EAR, DF = EARr.next(), DFr.next()
                self.act(EAR[:], psr[:, :], AF.Exp, reads=[psr], writes=[EAR])
                if L3 < 3.6:
                    continue
                for h in range(4):
                    self.stt(DF[:, h * 128:(h + 1) * 128], psr[:, h * 128:(h + 1) * 128], DT[:, 12 + h:13 + h], negmask,
                             ALU.subtract, ALU.add, reads=[psr, DT, self.CONST], writes=[DF])
                if L3 < 3.8:
                    continue
                self.act(DAB[:], DF[:], AF.Exp, reads=[DF], writes=[DAB])
                if L3 < 4:
                    continue
                psg = self.bank()
                for gi in range(2):
                    self.mm(psg[:, gi * 128:(gi + 1) * 128], BTb[:, gi, sl], CTb[:, sl], True, True, reads=[BTb, CTb], writes=[psg])
                if L3 < 4.2:
                    continue
                WT = WTr.next()
                for h in range(4):
                    gi = h // 2
                    self.tt(WT[:, h * 128:(h + 1) * 128], DAB[:, h * 128:(h + 1) * 128], psg[:, gi * 128:(gi + 1) * 128], ALU.mult,
                            reads=[DAB, psg], writes=[WT])
                if L3 < 4.4:
                    continue
                CST = CSTr.next()
                for h in range(4):
                    gi, hh = h // 2, h % 2
                    r_ = slice(64 * gi, 64 * gi + 64)
                    self.tt(CST[r_, h * 128:(h + 1) * 128], CTf[r_, sl], EAR[r_, h * 128:(h + 1) * 128], ALU.mult,
                            reads=[CTf, EAR], writes=[CST])
                if L3 < 5:
                    continue
                psy = self.bank()
                for h in range(4):
                    gi, hh = h // 2, h % 2
                    r_ = slice(64 * gi, 64 * gi + 64)
                    self.mm(psy[:, h * 64:(h + 1) * 64], WT[:, h * 128:(h + 1) * 128], XD[:, h * 64:(h + 1) * 64], True, False,
                            reads=[WT, XD], writes=[psy])
                    self.mm(psy[:, h * 64:(h + 1) * 64], CST[:, h * 128:(h + 1) * 128], Sb[:, hh * 64:(hh + 1) * 64], False, True,
                            reads=[CST, Sb], writes=[psy])
                if L3 < 6:
                    continue
                self.tt(DT[:, 16:20].unsqueeze(2), psr[:, :].rearrange("p (h i) -> p h i", h=4)[:, :, 127:128], DT[:, 12:16].unsqueeze(2),
                        ALU.subtract, reads=[psr, DT], writes=[DT])
                self.act(DT[:, 16:20], DT[:, 16:20], AF.Exp, reads=[DT], writes=[DT])
                self.tt(DT[:, 20:24], DT[:, 16:20], DT[:, 4:8], ALU.mult, reads=[DT], writes=[DT])
                self.tt(XDS[:, :].rearrange("p (h v) -> p h v", h=4), xs4, DT[:, 20:24].unsqueeze(2).to_broadcast([128, 4, 64]),
                        ALU.mult, reads=[XS, DT], writes=[XDS])
                pss = self.bank()
                self.mm(pss[:, 0:256], BK[:], XDS[:], True, True, reads=[BK, XDS], writes=[pss])
                for h in range(4):
                    gi, hh = h // 2, h % 2
                    r_ = slice(64 * gi, 64 * gi + 64)
                    self.stt(S32[r_, hh * 64:(hh + 1) * 64], S32[r_, hh * 64:(hh + 1) * 64], EAR[r_, h * 128 + 127:h * 128 + 128],
                             pss[r_, h * 64:(h + 1) * 64], ALU.mult, ALU.add, reads=[S32, EAR, pss], writes=[S32])
                self.cp(Sb[:], S32[:], reads=[S32], writes=[Sb], eng="pool")
                if L3 < 7:
                    continue
                Y1, SQ, ST, Y = Y1r.next(), SQr.next(), STr.next(), Yr.next()
                self.tt(Y1[:], XS[:], self.psm("ssm_d"), ALU.mult, reads=[XS, self.PSM], writes=[Y1], eng="pool")
                self.tt(Y1[:], Y1[:], psy[:, 0:256], ALU.add, reads=[Y1, psy], writes=[Y1])
                self.tt(Y1[:], Y1[:], SZ[:], ALU.mult, reads=[Y1, SZ], writes=[Y1])
                self.tt(SQ[:], Y1[:], Y1[:], ALU.mult, reads=[Y1], writes=[SQ], eng="pool")
                self.S.op("dve", lambda: nc.vector.tensor_reduce(out=ST[:, 0:2], in_=SQ[:, :].rearrange("p (g v) -> p g v", g=2),
                                                                 axis=AX.X, op=ALU.add), reads=[SQ], writes=[ST])
                self.act(ST[:, 0:2], ST[:, 0:2], AF.Sqrt, reads=[ST], writes=[ST], scale=1.0 / 128, bias=float(RMS_EPS))
                self.S.op("dve", lambda: nc.vector.reciprocal(out=ST[:, 4:6], in_=ST[:, 0:2]), reads=[ST], writes=[ST])
                self.tt(Y[:, :].rearrange("p (g v) -> p g v", g=2), Y1[:, :].rearrange("p (g v) -> p g v", g=2),
                        ST[:, 4:6].unsqueeze(2).to_broadcast([128, 2, 128]), ALU.mult, reads=[Y1, ST], writes=[Y])
                self.tt(Y[:], Y[:], self.psm("ssm_nw"), ALU.mult, reads=[Y, self.PSM], writes=[Y])
                self.emit_yT(Y, YTm, t)

    def rope_tables(self, s):
        cfg = self.cfg
        nc = self.nc
        NT = cfg.NT
        if not hasattr(self, "COS"):
            self.COS = self.alloc("COS", [128, NT, 8], F32)
            self.SIN = self.alloc("SIN", [128, NT, 8], F32)
        with ExitStack() as st:
            PI_ = self.alloc("rpI", [128, NT], I32, stack=st)
            PF = self.alloc("rpF", [128, NT], F32, stack=st)
            ANG = self.alloc("rpA", [128, NT, 8], F32, stack=st)
            TQ = self.alloc("rpQ", [128, NT, 8], F32, stack=st)
            KI = self.alloc("rpK", [128, NT, 8], I32, stack=st)
            KF = self.alloc("rpKF", [128, NT, 8], F32, stack=st)
            self.load(PI_[:], self.d_pos[s * 128:(s + 1) * 128, :], writes=[PI_])
            self.cp(PF[:], PI_[:], reads=[PI_], writes=[PF])
            self.tt(ANG[:], PF[:].unsqueeze(2).to_broadcast([128, NT, 8]), self.cst("invf").unsqueeze(1).to_broadcast([128, NT, 8]),
                    ALU.mult, reads=[PF, self.CONST], writes=[ANG])
            C1 = 6.28125
            C2 = 2.0 * math.pi - C1
            for dst, shift in ((self.SIN, 0.0), (self.COS, math.pi / 2)):
                src = ANG
                if shift != 0.0:
                    self.ts(TQ[:], ANG[:], shift, ALU.add, reads=[ANG], writes=[TQ])
                    src = TQ
                self.ts(KF[:], src[:], 1.0 / (2.0 * math.pi), ALU.mult, reads=[src], writes=[KF])
                self.cp(KI[:], KF[:], reads=[KF], writes=[KI])
                self.cp(KF[:], KI[:], reads=[KI], writes=[KF])
                self.stt(dst[:], KF[:], -C1, src[:], ALU.mult, ALU.add, reads=[KF, src], writes=[dst])
                self.stt(dst[:], KF[:], -C2, dst[:], ALU.mult, ALU.add, reads=[KF, dst], writes=[dst])
                self.ts(dst[:], dst[:], math.pi, ALU.min, reads=[dst], writes=[dst], s2=-math.pi, op1=ALU.max)
                self.act(dst[:], dst[:], AF.Sin, reads=[dst], writes=[dst])
            self.barrier()

    def mix_dil(self, s, l, st, YTm):
        cfg = self.cfg
        nc = self.nc
        T, NT = cfg.T, cfg.NT
        W = self.alloc("Wdil", [128, KD, 384], BF16, stack=st)
        QKT = self.alloc("dlQKT", [128, 2, T], BF16, stack=st)
        VP = [self.alloc(f"dlVP{i}", [128, NT, 128], BF16, stack=st) for i in range(3)]
        ACD = self.alloc("dlACD", [128, 2, T], F32, stack=st)
        NEGC = self.alloc("dlNEGC", [33, T], BF16, stack=st)
        ONESB = self.alloc("dlONES", [128, 128], BF16, stack=st)
        self.memset(ONESB[:], 1.0, writes=[ONESB])
        SEL = self.alloc("dlSEL", [128, 33], BF16, stack=st)
        self.memset(SEL[:], 0.0, writes=[SEL])
        self.memset(SEL[0:64, 0:1], 1.0, writes=[SEL])
        self.memset(SEL[64:128, 32:33], 1.0, writes=[SEL])
        KM = self.alloc("dlKM", [33, 8], F32, stack=st)
        RQr = self.tmp("dlRQ", [128, 256], F32, stack=st)
        T1r = self.tmp("dlT1", [128, 64], F32, stack=st)
        SQr = self.tmp("dlSQ", [128, 512], BF16, stack=st)
        C2r = self.tmp("dlC2", [33, 512], F32, stack=st)
        PTr = self.tmp("dlPT", [128, 256], BF16, n=4, stack=st)
        RD = self.alloc("dlRD", [128, 512], F32, stack=st)
        allXT = list(self.XT)
        dils = (1, 4, 16)
        for pair in range(2):
            for j in range(3):
                c0 = O_DQ + j * 256 + pair * 128
                self.loadw(W[:, :, j * 128:(j + 1) * 128], self.win_ap(l, c0, c0 + 128), writes=[W], stream="win")
            for t in range(NT):
                ps = self.bank()
                self.proj_tok(t, W, 0, 256, ps[:, 0:256], ps)
                RQ, T1 = RQr.next(), T1r.next()
                self.cp(RQ[:], ps[:, 0:256], reads=[ps], writes=[RQ], eng="act")
                pv = ps[:, 0:256].rearrange("p (h d) -> p h d", h=4)
                rv = RQ[:, :].rearrange("p (h d) -> p h d", h=4)
                cb = self.COS[:, t, :].unsqueeze(1).to_broadcast([128, 4, 8])
                sb = self.SIN[:, t, :].unsqueeze(1).to_broadcast([128, 4, 8])
                ta = T1[:, 0:32].rearrange("p (h d) -> p h d", h=4)
                tb = T1[:, 32:64].rearrange("p (h d) -> p h d", h=4)
                self.tt(ta, pv[:, :, 0:8], cb, ALU.mult, reads=[ps, self.COS], writes=[T1])
                self.tt(tb, pv[:, :, 8:16], sb, ALU.mult, reads=[ps, self.SIN], writes=[T1])
                self.tt(rv[:, :, 0:8], ta, tb, ALU.subtract, reads=[T1], writes=[RQ])
                self.tt(ta, pv[:, :, 8:16], cb, ALU.mult, reads=[ps, self.COS], writes=[T1])
                self.tt(tb, pv[:, :, 0:8], sb, ALU.mult, reads=[ps, self.SIN], writes=[T1])
                self.tt(rv[:, :, 8:16], ta, tb, ALU.add, reads=[T1], writes=[RQ])
                ps2 = self.bank()
                for j in range(2):
                    self.tr(ps2[:, j * 128:(j + 1) * 128], RQ[:, j * 128:(j + 1) * 128], reads=[RQ], writes=[ps2])
                self.cp(QKT[:, :, t * 128:(t + 1) * 128], ps2[:, 0:256].rearrange("p (a b) -> p a b", a=2), reads=[ps2], writes=[QKT],
                        eng="dve")
            for which in (1, 0):
                for g in range(cfg.NG):
                    SQ = SQr.next()
                    src = QKT[:, which, g * 512:(g + 1) * 512]
                    self.tt(SQ[:], src, src, ALU.mult, reads=[QKT], writes=[SQ], eng="pool")
                    psn = self.bank()
                    self.mm(psn[0:33, :], SEL[:], SQ[:], True, True, reads=[SEL, SQ], writes=[psn])
                    if which == 1:
                        self.S.op("dve", lambda: nc.vector.reduce_max(out=KM[:, g:g + 1], in_=psn[0:33, :], axis=AX.X), reads=[psn], writes=[KM])
                    else:
                        C2 = C2r.next()
                        self.ts(C2[:], psn[0:33, :], KM[:, 4:5], ALU.mult, reads=[psn, KM], writes=[C2])
                        self.act(C2[:], C2[:], AF.Sqrt, reads=[C2], writes=[C2])
                        self.ts(NEGC[:, g * 512:(g + 1) * 512], C2[:], -1.0, ALU.mult, reads=[C2], writes=[NEGC])
                if which == 1:
                    self.S.op("dve", lambda: nc.vector.reduce_max(out=KM[:, 4:5], in_=KM[:, 0:cfg.NG], axis=AX.X), reads=[KM], writes=[KM])
            for bi, dil in enumerate(dils):
                nb = NT // dil
                for r in range(dil):
                    for b in range(nb):
                        blk = r * nb + b
                        ps = self.bank()
                        for k in range(KD):
                            lhs = self.XTh[:, k, :].rearrange("p (l r) -> p r l", r=dil)[:, r, b * 128:(b + 1) * 128]
                            self.mm(ps[:, 0:128], lhs, W[:, k, 256:384], k == 0, k == KD - 1,
                                    reads=allXT + [W], writes=[ps])
                        self.cp(VP[bi][:, blk, :], ps[:, 0:128], reads=[ps], writes=[VP[bi]], eng=("act" if blk % 2 == 0 else "dve"))
            for bi, dil in enumerate(dils):
                nb = NT // dil
                qv = QKT[:, 0, :].rearrange("p (l r) -> p r l", r=dil)
                kv = QKT[:, 1, :].rearrange("p (l r) -> p r l", r=dil)
                cv = NEGC[:, :].rearrange("p (l r) -> p r l", r=dil)
                av = ACD[:, :, :].rearrange("p a (l r) -> p a r l", r=dil)
                for r in range(dil):
                    Bq = None
                    for b in range(nb):
                        blk = r * nb + b
                        nq = 256 if b + 1 < nb else 128
                        PTs = []
                        for hh in range(2):
                            R_ = slice(64 * hh, 64 * hh + 64)
                            sr = 32 * hh
                            pss = self.bank()
                            self.mm(pss[:, 0:nq], kv[R_, r, b * 128:(b + 1) * 128], qv[R_, r, b * 128:b * 128 + nq], True, False,
                                    reads=[QKT], writes=[pss])
                            self.mm(pss[:, 0:nq], self.IDB[:], self.DMASK[:, 0:nq], False, False, reads=[self.IDB, self.DMASK], writes=[pss])
                            self.mm(pss[:, 0:nq], ONESB[sr:sr + 1, :], cv[sr:sr + 1, r, b * 128:b * 128 + nq], False, True,
                                    reads=[ONESB, NEGC], writes=[pss])
                            PT = PTr.next()
                            self.act(PT[:, 0:nq], pss[:, 0:nq], AF.Exp, reads=[pss], writes=[PT], scale=0.125)
                            PTs.append(PT)
                        if Bq is None:
                            Bq = (self.bank(), self.bank())
                        for hh in range(2):
                            R_ = slice(64 * hh, 64 * hh + 64)
                            self.mm(Bq[0][R_, 0:128], VP[bi][:, blk, R_], PTs[hh][:, 0:128], b == 0, True, reads=[VP[bi], PTs[hh]], writes=[Bq[0]])
                            self.mm(Bq[1][R_, 0:128], ONESB[:, 0:64], PTs[hh][:, 0:128], b == 0, True, reads=[ONESB, PTs[hh]], writes=[Bq[1]])
                        for a in range(2):
                            dst = av[:, a, r, b * 128:(b + 1) * 128]
                            src = Bq[a][:, 0:128]
                            if bi == 0:
                                self.cp(dst, src, reads=[Bq[a]], writes=[ACD], eng=("dve" if a == 0 else "act"))
                            else:
                                self.tt(dst, src, dst, ALU.add, reads=[Bq[a], ACD], writes=[ACD])
                        Bq = None
                        if b + 1 < nb:
                            Bq = (self.bank(), self.bank())
                            for hh in range(2):
                                R_ = slice(64 * hh, 64 * hh + 64)
                                self.mm(Bq[0][R_, 0:128], VP[bi][:, blk, R_], PTs[hh][:, 128:256], True, False, reads=[VP[bi], PTs[hh]], writes=[Bq[0]])
                                self.mm(Bq[1][R_, 0:128], ONESB[:, 0:64], PTs[hh][:, 128:256], True, False, reads=[ONESB, PTs[hh]], writes=[Bq[1]])
            for g in range(cfg.NG):
                sl = slice(g * 512, (g + 1) * 512)
                self.S.op("dve", lambda: nc.vector.reciprocal(out=RD[:], in_=ACD[:, 1, sl]), reads=[ACD], writes=[RD])
                self.tt(YTm[:, pair, sl], ACD[:, 0, sl], RD[:], ALU.mult, reads=[ACD, RD], writes=[YTm])

    def outproj_ln_router(self, s, l, st):
        cfg = self.cfg
        nc = self.nc
        WO = self.alloc("WO", [128, KD, D], BF16, stack=st)
        self.loadw(WO[:], self.d_wout[l * D:(l + 1) * D, :].rearrange("(k p) n -> p k n", p=128), writes=[WO], stream="win")
        RW = self.alloc("RW", [128, KD, NE], F32, stack=st)
        self.load(RW[:], self.d_rw[l * D:(l + 1) * D, :].rearrange("(k p) n -> p k n", p=128), writes=[RW])
        LNP = self.alloc("LNP", [128, 2048], F32, stack=st)
        self.load(LNP[:], self.d_pvec[l * 128:(l + 1) * 128, 0:2048], writes=[LNP])
        Tt = self.tmp("Tt", [128, D], F32, stack=st)
        Tn = self.tmp("Tn", [128, D], F32, stack=st)
        STt = self.tmp("lnS", [128, 8], F32, stack=st)
        HTf = self.tmp("HTf", [128, KD, 128], F32, stack=st)
        R = {k: self.tmp("rt" + k, [128, w], F32, stack=st) for k, w in
             [("sc", 128), ("ch", 128), ("m8", 64), ("gs", 8), ("m8b", 8), ("gm", 8), ("pen", 8), ("cm", 128),
              ("m8c", 8), ("sel", 128), ("gu", 128), ("sm", 2)]}
        def tile(t, par):
            psA, psB = self.bank(), self.bank()
            for half, ps in enumerate((psA, psB)):
                for k in range(KD):
                    m = k // 2
                    self.mm(ps[:, :], self.YTh[:, k, t * 128:(t + 1) * 128], WO[:, k, half * 512:(half + 1) * 512],
                            k == 0, k == KD - 1, reads=[self.YT[m], WO], writes=[ps], inc=(k == KD - 1))
            T_, TN, ST = Tt.bufs[par], Tn.bufs[par], STt.bufs[par]
            for half, ps in enumerate((psA, psB)):
                sl = slice(half * 512, (half + 1) * 512)
                self.stt(T_[:, sl], self.X[t][:, sl], ALPHA, ps[:, :], ALU.mult, ALU.add, reads=[self.X[t], ps], writes=[T_])
            self.layernorm(T_[:], self.X[t][:], LNP[:, 0:1024], LNP[:, 1024:2048], D, LN_EPS, reads=[T_], preads=[LNP],
                           writes=[self.X[t]], st=ST, tmpn=TN)
            if cfg.dbg and s == 0 and l == 0:
                self.load(self.dbg["dbg_h"][t * 128:(t + 1) * 128, :], self.X[t][:], writes=[], reads=[self.X[t]], stream="dbg")
            import os
            lvl = int(os.environ.get("STOP2", "9"))
            if lvl < 1:
                return
            H = HTf.bufs[par]
            self.transpose_tile(t, htf=H)
            if lvl < 2:
                return
            psR = self.bank()
            for k in range(KD):
                self.mm(psR[:, 0:NE], H[:, k, :], RW[:, k, :], k == 0, k == KD - 1, reads=[H, RW], writes=[psR])
            if lvl < 3:
                return
            self.routing(t, psR, R, par)
            if lvl < 4:
                return
            if cfg.dbg and s == 0 and l == 0:
                self.load(self.dbg["dbg_G"][t * 128:(t + 1) * 128, :], self.Gh[:, t, 0:NE], writes=[], reads=[self.G], stream="dbg")
            self.ts(self.X[t][:], self.X[t][:], ALPHA, ALU.mult, reads=[self.X[t]], writes=[self.X[t]], eng="pool")
        self.zip_run([lambda p=p: [tile(t, p) for t in range(p, cfg.NT, 2)] for p in range(2)])

    def routing(self, t, psR, R, par):
        nc = self.nc
        sc, ch, m8, gs, m8b, gm, pen, cm, m8c, sel, gu, sm = [R[k].bufs[par] for k in
                                                               ("sc", "ch", "m8", "gs", "m8b", "gm", "pen", "cm", "m8c", "sel", "gu", "sm")]
        self.act(sc[:], psR[:, 0:NE], AF.Sigmoid, reads=[psR], writes=[sc])
        self.tt(ch[:], sc[:], self.psm("r_bias"), ALU.add, reads=[sc, self.PSM], writes=[ch])
        for g in range(8):
            self.S.op("dve", lambda: nc.vector.max(out=m8[:, g * 8:(g + 1) * 8], in_=ch[:, g * 16:(g + 1) * 16]), reads=[ch], writes=[m8])
        m8v = m8[:, :].rearrange("p (g k) -> p g k", g=8)
        self.tt(gs[:].unsqueeze(2), m8v[:, :, 0:1], m8v[:, :, 1:2], ALU.add, reads=[m8], writes=[gs])
        self.S.op("dve", lambda: nc.vector.max(out=m8b[:], in_=gs[:]), reads=[gs], writes=[m8b])
        self.ts(gm[:], gs[:], m8b[:, 3:4], ALU.is_ge, reads=[gs, m8b], writes=[gm])
        self.ts(pen[:], gm[:], 4.0, ALU.mult, reads=[gm], writes=[pen], s2=-4.0, op1=ALU.add)
        chv = ch[:, :].rearrange("p (g k) -> p g k", g=8)
        cmv = cm[:, :].rearrange("p (g k) -> p g k", g=8)
        self.tt(cmv, chv, gm[:].unsqueeze(2).to_broadcast([128, 8, 16]), ALU.mult, reads=[ch, gm], writes=[cm])
        self.tt(cmv, cmv, pen[:].unsqueeze(2).to_broadcast([128, 8, 16]), ALU.add, reads=[cm, pen], writes=[cm])
        self.S.op("dve", lambda: nc.vector.max(out=m8c[:], in_=cm[:]), reads=[cm], writes=[m8c])
        self.ts(sel[:], cm[:], m8c[:, 7:8], ALU.is_ge, reads=[cm, m8c], writes=[sel])
        self.tt(gu[:], sc[:], sel[:], ALU.mult, reads=[sc, sel], writes=[gu])
        self.S.op("dve", lambda: nc.vector.reduce_sum(out=sm[:, 0:1], in_=gu[:], axis=AX.X), reads=[gu], writes=[sm])
        self.S.op("dve", lambda: nc.vector.reciprocal(out=sm[:, 1:2], in_=sm[:, 0:1]), reads=[sm], writes=[sm])
        self.ts(self.Gh[:, t, 0:NE], gu[:], sm[:, 1:2], ALU.mult, reads=[gu, sm], writes=[self.G])

    def exp_w_aps(self, l, e):
        if e < NE:
            r = (l * NE + e) * D
            wg = self.d_wg[r:r + D, :]
            wu = self.d_wu[r:r + D, :]
            r2 = (l * NE + e) * 256
            wd = self.d_wd[r2:r2 + 256, :]
        else:
            wg = self.d_swg[l * D:(l + 1) * D, :]
            wu = self.d_swu[l * D:(l + 1) * D, :]
            wd = self.d_swd[l * 256:(l + 1) * 256, :]
        return (wg.rearrange("(k p) n -> p k n", p=128), wu.rearrange("(k p) n -> p k n", p=128),
                wd.rearrange("(k p) n -> p k n", p=128))

    def moe(self, s, l, st):
        cfg = self.cfg
        nc = self.nc
        NB = 3
        WGU = [self.alloc(f"WGU{i}", [128, KD, 512], BF16, stack=st) for i in range(NB)]
        WD = [self.alloc(f"WD{i}", [128, 2, D], BF16, stack=st) for i in range(NB)]
        AT = [self.alloc(f"AT{i}", [128, 2, cfg.T], BF16, stack=st) for i in range(2)]
        SG = self.tmp("SG", [128, 512], F32, n=3, stack=st)
        elist = list(range(cfg.n_exp - 1)) + [NE]

        def issue(i):
            e = elist[i]
            wg, wu, wd = self.exp_w_aps(l, e)
            b = i % NB
            self.loadw(WGU[b][:, :, 0:256], wg, writes=[WGU[b]], stream=f"we{b}", rot=1, chain=False)
            self.loadw(WGU[b][:, :, 256:512], wu, writes=[WGU[b]], stream=f"we{b}", rot=1, chain=False)
            self.loadw(WD[b][:], wd, writes=[WD[b]], stream=f"we{b}", rot=1, chain=False)
        for i in range(min(NB - 1, len(elist))):
            issue(i)
        for i, e in enumerate(elist):
            if i + NB - 1 < len(elist):
                issue(i + NB - 1)
            b = i % NB
            A = AT[i % 2]
            for c in range(2):
                for g in range(cfg.NG):
                    psg, psu = self.bank(), self.bank()
                    self.proj_feat(g, WGU[b], c * 128, 128, psg[:, :], psg)
                    self.proj_feat(g, WGU[b], 256 + c * 128, 128, psu[:, :], psu)
                    sg = SG.next()
                    self.act(sg[:], psg[:, :], AF.Silu, reads=[psg], writes=[sg])
                    self.tt(A[:, c, g * 512:(g + 1) * 512], psu[:, :], sg[:], ALU.mult, reads=[psu, sg], writes=[A])
            for t in range(cfg.NT):
                py = (self.bank(), self.bank())
                for half in range(2):
                    for c in range(2):
                        self.mm(py[half][:, :], A[:, c, t * 128:(t + 1) * 128], WD[b][:, c, half * 512:(half + 1) * 512],
                                c == 0, c == 1, reads=[A, WD[b]], writes=[py[half]], inc=(c == 1))
                for half in range(2):
                    sl = slice(half * 512, (half + 1) * 512)
                    self.stt(self.X[t][:, sl], py[half][:, :], self.Gh[:, t, e:e + 1], self.X[t][:, sl], ALU.mult, ALU.add,
                             reads=[py[half], self.G, self.X[t]], writes=[self.X[t]])
        LNP = self.alloc("LNP2", [128, 2048], F32, stack=st)
        self.load(LNP[:], self.d_pvec[l * 128:(l + 1) * 128, 2048:4096], writes=[LNP])
        Tn = self.tmp("Tn2", [128, D], F32, stack=st)
        STt = self.tmp("lnS2", [128, 8], F32, stack=st)
        last = (l == cfg.NL - 1)
        def tile2(t, par):
            self.layernorm(self.X[t][:], self.X[t][:], LNP[:, 0:1024], LNP[:, 1024:2048], D, LN_EPS, reads=[self.X[t]],
                           preads=[LNP], writes=[self.X[t]], st=STt.bufs[par], tmpn=Tn.bufs[par])
            if last:
                r0 = s * cfg.T + t * 128
                self.load(self.d_out[r0:r0 + 128, :], self.X[t][:], writes=[], reads=[self.X[t]], stream="out")
            else:
                self.transpose_tile(t)
        self.zip_run([lambda p=p: [tile2(t, p) for t in range(p, cfg.NT, 2)] for p in range(2)])


def prep_core_inputs(inp, cfg, core):
    NL, T, NSEQ, NT = cfg.NL, cfg.T, cfg.NSEQ, cfg.NT
    f = lambda a: np.ascontiguousarray(np.asarray(a))
    seqs = [core * NSEQ + i for i in range(NSEQ)]
    x = f(inp["x"])[seqs][:, :T].reshape(NSEQ * T, D)
    pos = f(inp["positions"])[seqs][:, :T].reshape(NSEQ, NT, 128).transpose(0, 2, 1).reshape(NSEQ * 128, NT)
    m = {"x": x, "pos": np.ascontiguousarray(pos).astype(np.int32)}
    m["w_in"] = f(inp["w_in"])[:NL].reshape(NL * D, NIN)
    m["w_out"] = f(inp["w_out"])[:NL].reshape(NL * D, D)
    m["router_w"] = f(inp["router_w"])[:NL].reshape(NL * D, NE)
    import os
    if os.environ.get("STOP", "") in ("mix", "load"):
        m["exp_w_gate"] = np.zeros((8, 256), np.float32); m["exp_w_up"] = np.zeros((8, 256), np.float32)
        m["exp_w_down"] = np.zeros((8, D), np.float32)
    else:
        m["exp_w_gate"] = f(inp["exp_w_gate"])[:NL].reshape(NL * NE * D, 256)
        m["exp_w_up"] = f(inp["exp_w_up"])[:NL].reshape(NL * NE * D, 256)
        m["exp_w_down"] = f(inp["exp_w_down"])[:NL].reshape(NL * NE * 256, D)
    m["sh_w_gate"] = f(inp["sh_w_gate"])[:NL].reshape(NL * D, 256)
    m["sh_w_up"] = f(inp["sh_w_up"])[:NL].reshape(NL * D, 256)
    m["sh_w_down"] = f(inp["sh_w_down"])[:NL].reshape(NL * 256, D)
    m["gla_w_gate"] = f(inp["gla_w_gate"])[:NL].reshape(NL * 16, 128)
    m["sgu_wT"] = np.ascontiguousarray(f(inp["sgu_w"])[:NL].transpose(0, 1, 3, 2)).reshape(NL * 4 * 128, 128)
    pv = np.zeros((NL, 128, NPV), np.float32)

    def put(name, arr):
        o, w = PV[name]
        pv[:, :, o:o + w] = arr[:, None, :]
    put("ln1_g", f(inp["ln1_g"])[:NL]); put("ln1_b", f(inp["ln1_b"])[:NL])
    put("ln2_g", f(inp["ln2_g"])[:NL]); put("ln2_b", f(inp["ln2_b"])[:NL])
    put("sgu_g", f(inp["sgu_ln_g"])[:NL]); put("sgu_b", f(inp["sgu_ln_b"])[:NL])
    put("gla_nw", np.tile(f(inp["gla_norm_w"])[:NL], (1, 4)))
    put("ssm_nw", f(inp["ssm_norm_w"])[:NL])
    put("ssm_d", np.repeat(f(inp["ssm_d"])[:NL], 64, axis=1))
    put("dt_bias", f(inp["ssm_dt_bias"])[:NL]); put("a_log", f(inp["ssm_a_log"])[:NL])
    put("r_bias", f(inp["router_bias"])[:NL])
    o, w = PV["sgu_bs"]
    sb = f(inp["sgu_b"])[:NL]
    pv[:, :, o:o + w] = np.repeat(sb.transpose(0, 2, 1), 64, axis=2)
    m["pvec"] = pv.reshape(NL * 128, NPV)
    pc = np.zeros((NL, 128, NPC), np.float32)
    pc[:, :, 0] = f(inp["gla_b_gate"])[:NL]
    cw = f(inp["ssm_conv_w"])[:NL]
    pc[:, :, 1:17] = cw.reshape(NL, 4, 4, 128).transpose(0, 3, 2, 1).reshape(NL, 128, 16)
    cb = f(inp["ssm_conv_b"])[:NL]
    pc[:, :, 17:21] = cb.reshape(NL, 4, 128).transpose(0, 2, 1)
    m["pcol"] = pc.reshape(NL * 128, NPC)
    m["consts"] = make_consts()
    return m


_NC_CACHE = {}


def kernel(**inputs):
    from concourse.bass_utils import run_bass_kernel_spmd
    n_cores = 8
    cfg = Cfg(T=2048, NSEQ=2, NL=4, n_exp=129)
    if "nc" not in _NC_CACHE:
        _NC_CACHE["nc"] = MK(cfg).build()
    nc = _NC_CACHE["nc"]
    inp = {k: np.asarray(v) for k, v in inputs.items()}
    shared = None
    in_maps = []
    for c in range(n_cores):
        if shared is None:
            m = prep_core_inputs(inp, cfg, c)
            shared = m
        else:
            m = dict(shared)
            seqs = [c * cfg.NSEQ + i for i in range(cfg.NSEQ)]
            m["x"] = np.ascontiguousarray(inp["x"][seqs].reshape(cfg.NSEQ * cfg.T, D))
            pos = inp["positions"][seqs].reshape(cfg.NSEQ, cfg.NT, 128).transpose(0, 2, 1).reshape(cfg.NSEQ * 128, cfg.NT)
            m["pos"] = np.ascontiguousarray(pos).astype(np.int32)
        in_maps.append(m)
    res = run_bass_kernel_spmd(nc, in_maps, core_ids=list(range(n_cores)))
    outs = [np.asarray(r["out"]).reshape(cfg.NSEQ, cfg.T, D) for r in res.results]
    return np.concatenate(outs, axis=0).astype(np.float32)
```
